# Optimizing a Trainium2 kernel written in Bass

```python
import jax, jax.numpy as jnp
from jax import lax
import numpy as np


D_MODEL = 1024
BATCH = 2
SEQ = 8192
DEPTH = 1

GRID_W = 64
CTX_LEN = 256
EPS = 1e-6

MLSTM_HEADS = 4
MLSTM_DH = 128
MLSTM_W = MLSTM_HEADS * MLSTM_DH
MLSTM_GATES = 2 * 2 * MLSTM_HEADS
MLSTM_CHUNK = 128

ATTN_Q_HEADS = 8
ATTN_KV_HEADS = 2
ATTN_GROUP = ATTN_Q_HEADS // ATTN_KV_HEADS
ATTN_DH = 64
ATTN_Q_W = ATTN_Q_HEADS * ATTN_DH
ATTN_KV_W = ATTN_KV_HEADS * ATTN_DH
WINDOW = 128
ATTN_BLOCK = 128
ROPE_BASE = 10000.0
ROPE_AXIS_FREQS = ATTN_DH // 4

N_BRANCH = 2
IN_SIZES = (MLSTM_W, MLSTM_W, MLSTM_W, MLSTM_W, MLSTM_GATES,
            ATTN_Q_W, ATTN_KV_W, ATTN_KV_W, N_BRANCH * D_MODEL)
D_IN = 4 * MLSTM_W + MLSTM_GATES + ATTN_Q_W + 2 * ATTN_KV_W + N_BRANCH * D_MODEL

PEER_HEADS = 8
PEER_N_KEYS = 128
PEER_N_EXPERTS = PEER_N_KEYS * PEER_N_KEYS
PEER_KEY_DIM = 256
PEER_HALF = PEER_KEY_DIM // 2
PEER_TOPK = 16
PEER_BLOCK = 128

kernel_name = 'hybrid_mlstm_swa_peer_dit_block'


def _rms(x, g):
    xf = x.astype(jnp.float32)
    y = xf * lax.rsqrt(jnp.mean(xf * xf, axis=-1, keepdims=True) + EPS)
    return y.astype(x.dtype) * g


def _modulate(x, g, shift, scale):
    return _rms(x, g) * (1 + scale) + shift


def _adaln(cond, w, b):
    mod = jax.nn.silu(cond) @ w + b
    return tuple(m[:, None, :] for m in jnp.split(mod, 6, axis=-1))


def _split_in(p):
    idx = []
    acc = 0
    for s in IN_SIZES[:-1]:
        acc += s
        idx.append(acc)
    return jnp.split(p, idx, axis=-1)


def _axial_rope(T):
    rows = T // GRID_W
    row = jnp.broadcast_to(jnp.arange(rows)[:, None], (rows, GRID_W)).reshape(T)
    col = jnp.broadcast_to(jnp.arange(GRID_W)[None, :], (rows, GRID_W)).reshape(T)
    inv = ROPE_BASE ** (-jnp.arange(ROPE_AXIS_FREQS, dtype=jnp.float32) / ROPE_AXIS_FREQS)
    ang = jnp.concatenate([row[:, None].astype(jnp.float32) * inv,
                           col[:, None].astype(jnp.float32) * inv], axis=-1)
    return jnp.cos(ang), jnp.sin(ang)


def _rope(t, cos, sin):
    t1, t2 = jnp.split(t, 2, axis=-1)
    c = cos[None, :, None, :]
    s = sin[None, :, None, :]
    return jnp.concatenate([t1 * c - t2 * s, t2 * c + t1 * s], axis=-1).astype(t.dtype)


def _chunk_seq(t, reverse):
    if reverse:
        t = jnp.flip(t, axis=1)
    B, T = t.shape[:2]
    t = jnp.moveaxis(t, 2, 1)
    return t.reshape((B, t.shape[1], T // MLSTM_CHUNK, MLSTM_CHUNK) + t.shape[3:])


def _unchunk(h, reverse):
    B, H = h.shape[:2]
    h = jnp.moveaxis(h.reshape(B, H, -1, h.shape[-1]), 1, 2)
    return jnp.flip(h, axis=1) if reverse else h


def _mlstm_summaries(k, v, li, lf):
    b = jnp.cumsum(lf, axis=-1)
    a = b[..., -1]
    w_log = a[..., None] - b + li
    m_loc = jnp.max(w_log, axis=-1)
    w = jnp.exp(w_log - m_loc[..., None])
    C_loc = jnp.einsum('bhcs,bhcsk,bhcsv->bhckv', w, k, v)
    n_loc = jnp.einsum('bhcs,bhcsk->bhck', w, k)
    return b, a, m_loc, C_loc, n_loc


def _mlstm_scan(a, m_loc, C_loc, n_loc, state0):
    def step(carry, inp):
        C, n, m = carry
        a_c, m_c, C_c, n_c = inp
        m_new = jnp.maximum(a_c + m, m_c)
        d_old = jnp.exp(a_c + m - m_new)
        d_new = jnp.exp(m_c - m_new)
        C_new = d_old[..., None, None] * C + d_new[..., None, None] * C_c
        n_new = d_old[..., None] * n + d_new[..., None] * n_c
        return (C_new, n_new, m_new), (C, n, m)
    xs = tuple(jnp.moveaxis(t, 2, 0) for t in (a, m_loc, C_loc, n_loc))
    final, starts = lax.scan(step, state0, xs)
    starts = tuple(jnp.moveaxis(t, 0, 2) for t in starts)
    return starts, final


def _mlstm_outputs(q, k, v, li, b, starts):
    C_s, n_s, m_s = starts
    L = q.shape[-2]
    log_inter = b + m_s[..., None]
    log_d = b[..., :, None] - b[..., None, :] + li[..., None, :]
    seen = jnp.tril(jnp.ones((L, L), dtype=bool))
    log_d = jnp.where(seen, log_d, -jnp.inf)
    m_t = jnp.maximum(log_inter, jnp.max(log_d, axis=-1))
    s = jnp.einsum('bhctk,bhcsk->bhcts', q, k) * jnp.exp(log_d - m_t[..., None])
    inter = jnp.exp(log_inter - m_t)
    num = (jnp.einsum('bhcts,bhcsv->bhctv', s, v)
           + inter[..., None] * jnp.einsum('bhctk,bhckv->bhctv', q, C_s))
    den = jnp.sum(s, axis=-1) + inter * jnp.einsum('bhctk,bhck->bhct', q, n_s)
    return num / jnp.maximum(jnp.abs(den), jnp.exp(-m_t))[..., None]


def _mlstm_direction(ctx_in, lat_in, reverse, with_ctx_out):
    qc, kc, vc, ic, fc = (_chunk_seq(t, reverse) for t in ctx_in)
    ql, kl, vl, il, fl = (_chunk_seq(t, reverse) for t in lat_in)
    B, H = qc.shape[:2]
    state0 = (jnp.zeros((B, H, MLSTM_DH, MLSTM_DH), jnp.float32),
              jnp.zeros((B, H, MLSTM_DH), jnp.float32),
              jnp.zeros((B, H), jnp.float32))
    bc, ac, mc, Cc, nc = _mlstm_summaries(kc, vc, ic, fc)
    starts_c, final_c = _mlstm_scan(ac, mc, Cc, nc, state0)
    bl, al, ml, Cl, nl = _mlstm_summaries(kl, vl, il, fl)
    starts_l, _ = _mlstm_scan(al, ml, Cl, nl, final_c)
    h_lat = _unchunk(_mlstm_outputs(ql, kl, vl, il, bl, starts_l), reverse)
    h_ctx = _unchunk(_mlstm_outputs(qc, kc, vc, ic, bc, starts_c), reverse) if with_ctx_out else None
    return h_lat, h_ctx


def _mlstm_branch(parts_lat, parts_ctx, b_mgates, norm_g, with_ctx_out):
    def prep(parts):
        q, k, v, o, gates = parts
        B, T = q.shape[:2]
        hd = lambda t: t.astype(jnp.float32).reshape(B, T, MLSTM_HEADS, MLSTM_DH)
        g = (gates.astype(jnp.float32) + b_mgates.astype(jnp.float32)).reshape(B, T, 2, 2, MLSTM_HEADS)
        return hd(q), hd(k) * (MLSTM_DH ** -0.5), hd(v), o, g

    ql, kl, vl, ol, gl = prep(parts_lat)
    qc, kc, vc, oc, gc = prep(parts_ctx)
    hs_lat, hs_ctx = [], []
    for d, reverse in enumerate((False, True)):
        lat_in = (ql, kl, vl, gl[:, :, d, 0], jax.nn.log_sigmoid(gl[:, :, d, 1]))
        ctx_in = (qc, kc, vc, gc[:, :, d, 0], jax.nn.log_sigmoid(gc[:, :, d, 1]))
        hl, hc = _mlstm_direction(ctx_in, lat_in, reverse, with_ctx_out)
        hs_lat.append(hl)
        hs_ctx.append(hc)

    def finish(h, o):
        B, T = h.shape[:2]
        hn = h * lax.rsqrt(jnp.mean(h * h, axis=-1, keepdims=True) + EPS)
        return hn.reshape(B, T, MLSTM_W).astype(o.dtype) * norm_g * jax.nn.sigmoid(o)

    m_lat = finish(hs_lat[0] + hs_lat[1], ol)
    m_ctx = finish(hs_ctx[0] + hs_ctx[1], oc) if with_ctx_out else None
    return m_lat, m_ctx


def _softmax_with_sink(logits, sink_b):
    sink_col = jnp.broadcast_to(sink_b, logits.shape[:-1] + (1,))
    return jax.nn.softmax(jnp.concatenate([logits, sink_col], axis=-1), axis=-1)[..., :-1]


def _window_attention(q, k, v, kc, vc, sink):
    B, T = q.shape[:2]
    nb = T // ATTN_BLOCK
    scale = ATTN_DH ** -0.5
    qb = q.reshape(B, nb, ATTN_BLOCK, ATTN_KV_HEADS, ATTN_GROUP, ATTN_DH)

    def band(t):
        tp = jnp.pad(t, ((0, 0), (ATTN_BLOCK, ATTN_BLOCK), (0, 0), (0, 0)))
        tp = tp.reshape(B, nb + 2, ATTN_BLOCK, ATTN_KV_HEADS, ATTN_DH)
        return jnp.concatenate([tp[:, :-2], tp[:, 1:-1], tp[:, 2:]], axis=2)

    kw, vw = band(k), band(v)
    qi = jnp.arange(ATTN_BLOCK)[:, None]
    kj = jnp.arange(3 * ATTN_BLOCK)[None, :]
    key_pos = jnp.arange(nb)[:, None, None] * ATTN_BLOCK - ATTN_BLOCK + kj[None]
    mask = (jnp.abs(kj - ATTN_BLOCK - qi) <= WINDOW)[None] & (key_pos >= 0) & (key_pos < T)
    s_loc = jnp.einsum('bnqhgd,bnkhd->bnhgqk', qb, kw).astype(jnp.float32) * scale
    s_loc = jnp.where(mask[None, :, None, None], s_loc, -jnp.inf)
    s_ctx = jnp.einsum('bnqhgd,bchd->bnhgqc', qb, kc).astype(jnp.float32) * scale
    sink_b = sink.astype(jnp.float32).reshape(ATTN_KV_HEADS, ATTN_GROUP, 1, 1)
    p = _softmax_with_sink(jnp.concatenate([s_loc, s_ctx], axis=-1), sink_b)
    p_loc = p[..., :3 * ATTN_BLOCK].astype(v.dtype)
    p_ctx = p[..., 3 * ATTN_BLOCK:].astype(v.dtype)
    out = (jnp.einsum('bnhgqk,bnkhd->bnqhgd', p_loc, vw)
           + jnp.einsum('bnhgqc,bchd->bnqhgd', p_ctx, vc))
    return out.reshape(B, T, ATTN_Q_W)


def _context_attention(qc, kc, vc, sink):
    B, Lc = qc.shape[:2]
    qg = qc.reshape(B, Lc, ATTN_KV_HEADS, ATTN_GROUP, ATTN_DH)
    s = jnp.einsum('bqhgd,bkhd->bhgqk', qg, kc).astype(jnp.float32) * (ATTN_DH ** -0.5)
    sink_b = sink.astype(jnp.float32).reshape(ATTN_KV_HEADS, ATTN_GROUP, 1, 1)
    p = _softmax_with_sink(s, sink_b).astype(vc.dtype)
    return jnp.einsum('bhgqk,bkhd->bqhgd', p, vc).reshape(B, Lc, ATTN_Q_W)


def _attn_branch(parts_lat, parts_ctx, q_g, k_g, sink, cos, sin, with_ctx_out):
    def heads(t, h):
        return t.reshape(t.shape[0], t.shape[1], h, ATTN_DH)
    ql, kl, vl = parts_lat
    qc, kc, vc = parts_ctx
    ql = _rope(_rms(heads(ql, ATTN_Q_HEADS), q_g), cos, sin)
    kl = _rope(_rms(heads(kl, ATTN_KV_HEADS), k_g), cos, sin)
    vl = heads(vl, ATTN_KV_HEADS)
    kc = _rms(heads(kc, ATTN_KV_HEADS), k_g)
    vc = heads(vc, ATTN_KV_HEADS)
    a_lat = _window_attention(ql, kl, vl, kc, vc, sink)
    a_ctx = None
    if with_ctx_out:
        a_ctx = _context_attention(_rms(heads(qc, ATTN_Q_HEADS), q_g), kc, vc, sink)
    return a_lat, a_ctx


def _token_mixer(h_lat, h_ctx, w_in, b_mgates, mlstm_norm_g, q_g, k_g, sink,
                 w_bm, w_ba, w_out, cos, sin, with_ctx_out):
    pl = _split_in(h_lat @ w_in)
    pc = _split_in(h_ctx @ w_in)
    m_lat, m_ctx = _mlstm_branch(pl[:5], pc[:5], b_mgates, mlstm_norm_g, with_ctx_out)
    a_lat, a_ctx = _attn_branch(pl[5:8], pc[5:8], q_g, k_g, sink, cos, sin, with_ctx_out)

    def merge(m, a, gates):
        g_m, g_a = jnp.split(gates, 2, axis=-1)
        return (jax.nn.sigmoid(g_m) * (m @ w_bm) + jax.nn.sigmoid(g_a) * (a @ w_ba)) @ w_out

    y_lat = merge(m_lat, a_lat, pl[8])
    y_ctx = merge(m_ctx, a_ctx, pc[8]) if with_ctx_out else None
    return y_lat, y_ctx


def _peer(h, w_query, sub_keys, expert_u, expert_v):
    B, T, D = h.shape
    tok = h.reshape(-1, PEER_BLOCK, D)

    def block(xb):
        q = (xb @ w_query).reshape(PEER_BLOCK, PEER_HEADS, 2, PEER_HALF)
        s = jnp.einsum('mhpd,hpnd->mhpn', q, sub_keys).astype(jnp.float32)
        s1, i1 = lax.top_k(s[:, :, 0], PEER_TOPK)
        s2, i2 = lax.top_k(s[:, :, 1], PEER_TOPK)
        cand = (s1[..., :, None] + s2[..., None, :]).reshape(PEER_BLOCK, PEER_HEADS, PEER_TOPK * PEER_TOPK)
        cidx = (i1[..., :, None] * PEER_N_KEYS + i2[..., None, :]).reshape(PEER_BLOCK, PEER_HEADS, PEER_TOPK * PEER_TOPK)
        top, pos = lax.top_k(cand, PEER_TOPK)
        idx = jnp.take_along_axis(cidx, pos, axis=-1)
        g = jax.nn.softmax(top, axis=-1)
        act = jax.nn.gelu(jnp.einsum('mhkd,md->mhk', expert_u[idx], xb).astype(jnp.float32))
        w = (g * act).astype(xb.dtype)
        return jnp.einsum('mhk,mhkd->md', w, expert_v[idx])

    return lax.map(block, tok).reshape(B, T, D)


def setup_inputs(seed: int = 0) -> dict:
    key = jax.random.key(seed)
    ks = jax.random.split(key, 24)
    f32 = jnp.float32

    def nrm(k, shape, s):
        return jax.random.normal(k, shape, f32) * s

    gate_base = jnp.tile(jnp.concatenate([jnp.zeros((MLSTM_HEADS,), f32),
                                          jnp.linspace(3.0, 6.0, MLSTM_HEADS, dtype=f32)]), 2)
    return {
        'x': nrm(ks[0], (BATCH, SEQ, D_MODEL), 1.0),
        'c': nrm(ks[1], (BATCH, D_MODEL), 1.0),
        'ctx': nrm(ks[2], (BATCH, CTX_LEN, D_MODEL), 1.0),
        'c_ctx': nrm(ks[3], (D_MODEL,), 1.0),
        'w_ada': nrm(ks[4], (DEPTH, D_MODEL, 6 * D_MODEL), D_MODEL ** -0.5),
        'b_ada': nrm(ks[5], (DEPTH, 6 * D_MODEL), 0.02),
        'norm_mix_g': 1.0 + nrm(ks[6], (DEPTH, D_MODEL), 0.02),
        'norm_ffn_g': 1.0 + nrm(ks[7], (DEPTH, D_MODEL), 0.02),
        'w_in': nrm(ks[8], (DEPTH, D_MODEL, D_IN), D_MODEL ** -0.5),
        'b_mgates': gate_base + nrm(ks[9], (DEPTH, MLSTM_GATES), 0.1),
        'mlstm_norm_g': 1.0 + nrm(ks[10], (DEPTH, MLSTM_W), 0.02),
        'attn_q_norm_g': 1.0 + nrm(ks[11], (DEPTH, ATTN_DH), 0.02),
        'attn_k_norm_g': 1.0 + nrm(ks[12], (DEPTH, ATTN_DH), 0.02),
        'attn_sink': nrm(ks[13], (DEPTH, ATTN_Q_HEADS), 0.5),
        'w_branch_m': nrm(ks[14], (DEPTH, MLSTM_W, D_MODEL), MLSTM_W ** -0.5),
        'w_branch_a': nrm(ks[15], (DEPTH, ATTN_Q_W, D_MODEL), ATTN_Q_W ** -0.5),
        'w_out': nrm(ks[16], (DEPTH, D_MODEL, D_MODEL), D_MODEL ** -0.5),
        'peer_w_query': nrm(ks[17], (DEPTH, D_MODEL, PEER_HEADS * PEER_KEY_DIM), D_MODEL ** -0.5),
        'peer_sub_keys': nrm(ks[18], (DEPTH, PEER_HEADS, 2, PEER_N_KEYS, PEER_HALF), PEER_HALF ** -0.5),
        'peer_u': nrm(ks[19], (DEPTH, PEER_N_EXPERTS, D_MODEL), D_MODEL ** -0.5),
        'peer_v': nrm(ks[20], (DEPTH, PEER_N_EXPERTS, D_MODEL), 0.5),
    }


def reference(x, c, ctx, c_ctx, w_ada, b_ada, norm_mix_g, norm_ffn_g, w_in, b_mgates,
              mlstm_norm_g, attn_q_norm_g, attn_k_norm_g, attn_sink, w_branch_m, w_branch_a,
              w_out, peer_w_query, peer_sub_keys, peer_u, peer_v):
    T = x.shape[1]
    cos, sin = _axial_rope(T)
    for layer in range(DEPTH):
        update_ctx = layer + 1 < DEPTH
        sh1, sc1, g1, sh2, sc2, g2 = _adaln(c, w_ada[layer], b_ada[layer])
        csh1, csc1, cg1, csh2, csc2, cg2 = _adaln(c_ctx[None], w_ada[layer], b_ada[layer])
        h_lat = _modulate(x, norm_mix_g[layer], sh1, sc1)
        h_ctx = _modulate(ctx, norm_mix_g[layer], csh1, csc1)
        y_lat, y_ctx = _token_mixer(h_lat, h_ctx, w_in[layer], b_mgates[layer], mlstm_norm_g[layer],
                                    attn_q_norm_g[layer], attn_k_norm_g[layer], attn_sink[layer],
                                    w_branch_m[layer], w_branch_a[layer], w_out[layer],
                                    cos, sin, update_ctx)
        x = x + g1 * y_lat
        x = x + g2 * _peer(_modulate(x, norm_ffn_g[layer], sh2, sc2), peer_w_query[layer],
                           peer_sub_keys[layer], peer_u[layer], peer_v[layer])
        if update_ctx:
            ctx = ctx + cg1 * y_ctx
            ctx = ctx + cg2 * _peer(_modulate(ctx, norm_ffn_g[layer], csh2, csc2), peer_w_query[layer],
                                    peer_sub_keys[layer], peer_u[layer], peer_v[layer])
    return x
```

```python
import contextlib
import numpy as np
import concourse.bass as bass
import concourse.mybir as mybir
from concourse.bass_utils import run_bass_kernel_spmd

F32 = mybir.dt.float32
BF16 = mybir.dt.bfloat16
U32 = mybir.dt.uint32
AF = mybir.ActivationFunctionType
ALU = mybir.AluOpType
AX = mybir.AxisListType

import os
ENGS = ("pe", "act", "dve", "pool", "sp")
REORDER_PHASES = set(os.environ.get("REORDER_PHASES", "").split(","))
REORDER_WINDOW = int(os.environ.get("REORDER_WINDOW", "100000"))
EPS = 1e-6
NPRE = 52
NOWN = 16
NEXT = 18
D = 1024
DIN = 4880
C_QM, C_KM, C_VM, C_OM, C_G, C_AQ, C_AK, C_AV, C_MG = 0, 512, 1024, 1536, 2048, 2064, 2576, 2704, 2832


class Sched:
    def __init__(self, nc, stack, n_dma_sems=48):
        self.nc = nc
        self.sems = {}
        names = ["s_" + e for e in ENGS if e != "sp"] + ["d%d" % i for i in range(n_dma_sems)]
        for n in names:
            self.sems[n] = stack.enter_context(nc.semaphore(n))
        self.engobj = {"pe": nc.tensor, "act": nc.scalar, "dve": nc.vector, "pool": nc.gpsimd, "sp": nc.sync}
        self.cnt = {e: 0 for e in ENGS}
        self.known = {e: {} for e in ENGS}
        self.pending = {e: {} for e in ENGS}
        self.bufs = {}
        self.n_dma_sems = n_dma_sems
        self.dma_next = 0
        self.dma_val = [0] * n_dma_sems
        self.dma_last_ev = [None] * n_dma_sems
        self.nops = 0
        self.reorder = False
        self.rec = []

    def set_reorder(self, flag):
        self.flush()
        self.reorder = flag

    def _b(self, name):
        b = self.bufs.get(name)
        if b is None:
            b = self.bufs[name] = {"w": None, "r": []}
        return b

    def latest_events(self):
        evs = []
        for e in ENGS:
            if e != "sp" and self.cnt[e] > 0:
                evs.append(("s_" + e, self.cnt[e]))
        for i in range(self.n_dma_sems):
            if self.dma_val[i] > 0:
                evs.append(("d%d" % i, self.dma_val[i]))
        return evs

    def barrier(self):
        self.flush()
        evs = self.latest_events()
        for e in ENGS:
            for (s, v) in evs:
                if self.pending[e].get(s, 0) < v:
                    self.pending[e][s] = v
        self.bufs = {}

    class _Probe:
        class _Ins:
            def then_inc(self, *a, **k):
                return self

        def __init__(self):
            self.calls = []

        def __getattr__(self, name):
            def f(*args, **kw):
                self.calls.append((name, args, kw))
                return Sched._Probe._Ins()
            return f

    @staticmethod
    def _free(ap):
        n = 1
        for d in ap.shape[1:]:
            n *= int(d)
        return n

    def _estimate(self, eng, call, dma):
        name, args, kw = call
        try:
            if dma:
                o = kw.get("out")
                nbytes = self._free(o) * int(o.shape[0]) * (4 if o.dtype == F32 or o.dtype == U32 else 2)
                return 0.12, 2.2 + nbytes / 1.2e5
            if name == "matmul":
                rhs = kw.get("rhs")
                n = self._free(rhs)
                t = 0.03 + max(n, 64) / 2000.0
                if rhs.dtype == F32:
                    t *= 4
                return t, t
            if name == "transpose":
                return 0.08, 0.08
            o = kw.get("out")
            if o is None:
                o = args[0]
            n = self._free(o)
            if eng == "dve":
                t = 0.2 + n / 960.0
            elif eng == "act":
                t = 0.22 + n / 1400.0 + (0.1 if kw.get("accum_out") is not None else 0.0)
            else:
                t = 0.3 + n / 500.0
            return t, t
        except Exception:
            return 0.5, 0.5

    def op(self, eng, fn, reads=(), writes=(), dma=False):
        if not self.reorder:
            return self._emit(eng, fn, reads, writes, dma)
        p = Sched._Probe()
        fn(p)
        assert len(p.calls) == 1
        call = p.calls[0]
        occ, lat = self._estimate(eng, call, dma)
        fn = (lambda e, c=call: getattr(e, c[0])(*c[1], **c[2]))
        reads, writes = tuple(reads), tuple(writes)
        if self.rec and eng == "pe" and not dma:
            last = self.rec[-1]
            if last[0] == "pe" and not last[4] and last[2] == reads and last[3] == writes:
                last[1].append(fn)
                last[5] += occ
                last[6] += lat
                return None
        self.rec.append([eng, [fn], reads, writes, dma, occ, lat])
        if len(self.rec) >= REORDER_WINDOW:
            self.flush()
        return None

    def flush(self):
        rec = self.rec
        self.rec = []
        n = len(rec)
        if n == 0:
            return
        import heapq
        succ = [[] for _ in range(n)]
        indeg = [0] * n
        lastw = {}
        readers = {}
        for i, (eng, fns, reads, writes, dma, occ, lat) in enumerate(rec):
            deps = set()
            for b in reads:
                if b in lastw:
                    deps.add(lastw[b])
            for b in writes:
                if b in lastw:
                    deps.add(lastw[b])
                for r_ in readers.get(b, ()):
                    deps.add(r_)
            deps.discard(i)
            for d in deps:
                succ[d].append(i)
            indeg[i] = len(deps)
            for b in reads:
                readers.setdefault(b, []).append(i)
            for b in writes:
                lastw[b] = i
                readers[b] = []
        ready = [0.0] * n
        free = {e: 0.0 for e in ENGS}
        fut = {e: [] for e in ENGS}
        now = {e: [] for e in ENGS}
        for i in range(n):
            if indeg[i] == 0:
                heapq.heappush(fut[rec[i][0]], (0.0, i))
        order = []
        while len(order) < n:
            best = None
            for e in ENGS:
                f, nw = fut[e], now[e]
                while f and f[0][0] <= free[e]:
                    heapq.heappush(nw, heapq.heappop(f)[1])
                if nw:
                    cand = (free[e], nw[0], e, True)
                elif f:
                    cand = (f[0][0], f[0][1], e, False)
                else:
                    continue
                if best is None or cand[:2] < best[:2]:
                    best = cand
            start, i, e, from_now = best
            if from_now:
                heapq.heappop(now[e])
            else:
                heapq.heappop(fut[e])
            order.append(i)
            free[e] = start + rec[i][5]
            done = start + rec[i][6]
            for j in succ[i]:
                rt = done + (0.1 if rec[j][0] == e else 0.2)
                if rt > ready[j]:
                    ready[j] = rt
                indeg[j] -= 1
                if indeg[j] == 0:
                    heapq.heappush(fut[rec[j][0]], (ready[j], j))
        if os.environ.get("REORDER_IDENTITY"):
            order = list(range(n))
        for i in order:
            eng, fns, reads, writes, dma, occ, lat = rec[i]
            self._emit(eng, fns, reads, writes, dma)

    def _emit(self, eng, fns, reads=(), writes=(), dma=False):
        if not isinstance(fns, (list, tuple)):
            fns = [fns]
        deps = set()
        for n in reads:
            b = self._b(n)
            if b["w"] is not None:
                deps.add(b["w"])
        for n in writes:
            b = self._b(n)
            if b["w"] is not None:
                deps.add(b["w"])
            for ev in b["r"]:
                deps.add(ev)
        for s, v in self.pending[eng].items():
            deps.add((s, v))
        self.pending[eng] = {}
        if dma:
            i = self.dma_next
            self.dma_next = (self.dma_next + 1) % self.n_dma_sems
            if self.dma_last_ev[i] is not None:
                deps.add(self.dma_last_ev[i])
            self.dma_val[i] += 16
            ev = ("d%d" % i, self.dma_val[i])
            self.dma_last_ev[i] = ev
            inc = 16
        else:
            self.cnt[eng] += 1
            ev = ("s_" + eng, self.cnt[eng])
            inc = 1
        waits = {}
        kn = self.known[eng]
        for (s, v) in deps:
            if eng == "pe" and s == "s_pe":
                continue
            if kn.get(s, 0) < v and waits.get(s, 0) < v:
                waits[s] = v
        e = self.engobj[eng]
        for (s, v) in sorted(waits.items()):
            kn[s] = v
            e.wait_ge(self.sems[s], v)
        for fn in fns[:-1]:
            fn(e)
        fns[-1](e).then_inc(self.sems[ev[0]], inc)
        self.nops += len(fns)
        for n in reads:
            self._b(n)["r"].append(ev)
        for n in writes:
            b = self._b(n)
            b["w"] = ev
            b["r"] = []
        return ev

    def finish(self):
        self.flush()
        e = self.engobj["sp"]
        for (s, v) in self.latest_events():
            e.wait_ge(self.sems[s], v)


class Ring:
    def __init__(self, K, name, shape, dt, n):
        self.t = [K.sb("%s%d" % (name, i), shape, dt) for i in range(n)]
        self.names = ["%s%d" % (name, i) for i in range(n)]
        self.n = n
        self.i = -1

    def next(self):
        self.i += 1
        return self.t[self.i % self.n], self.names[self.i % self.n]

    def cur(self):
        return self.t[self.i % self.n], self.names[self.i % self.n]


class Builder:
    def __init__(self, dbg=0):
        self.dbg = dbg
        self.nc = bass.Bass("TRN2", target_bir_lowering=False)
        self.root = contextlib.ExitStack()
        self.S = Sched(self.nc, self.root)
        self.scope = self.root

    def din(self, name, shape, dt=F32):
        return self.nc.dram_tensor(name, list(shape), dt, kind="ExternalInput").ap()

    def dout(self, name, shape, dt=F32):
        return self.nc.dram_tensor(name, list(shape), dt, kind="ExternalOutput").ap()

    def dscr(self, name, shape, dt=F32):
        return self.nc.dram_tensor(name, list(shape), dt).ap()

    def sb(self, name, shape, dt):
        return self.scope.enter_context(self.nc.sbuf_tensor(name, list(shape), dt))

    def ps(self, name, shape, dt):
        return self.scope.enter_context(self.nc.psum_tensor(name, list(shape), dt))

    def V(self, fn, r=(), w=()):
        return self.S.op("dve", fn, r, w)

    def A(self, fn, r=(), w=()):
        return self.S.op("act", fn, r, w)

    def G(self, fn, r=(), w=()):
        return self.S.op("pool", fn, r, w)

    def T(self, fn, r=(), w=()):
        return self.S.op("pe", fn, r, w)

    def DMA(self, fn, r=(), w=(), q="sp"):
        return self.S.op(q, fn, r, w, dma=True)


@contextlib.contextmanager
def phase_scope(K):
    with contextlib.ExitStack() as sc:
        yield sc
        K.S.flush()


def bc_mid(ap, n):
    return ap.unsqueeze(2).to_broadcast([ap.shape[0], ap.shape[1], n])


def build(dbg=0):
    K = Builder(dbg)
    nc, S = K.nc, K.S
    V, A, G, T, DMA = K.V, K.A, K.G, K.T, K.DMA

    xown = K.din("xown", [NEXT * 128, D])
    xpre = K.din("xpre", [NPRE * 128, D])
    pmask_d = K.din("pmask", [128, NPRE * 2])
    cT_d = K.din("cT", [128, 16])
    rope_d = K.din("rope", [128, NEXT * 64])
    amask_d = K.din("amask", [128, 256])
    consts_d = K.din("consts", [128, 3 * 128 + 16])
    nmgT_d = K.din("nmgT", [128, 8])
    w_ada = K.din("w_ada", [D, 6 * D])
    b_ada = K.din("b_ada", [1, 6 * D])
    nfg_d = K.din("norm_ffn_g", [1, D])
    w_in = K.din("w_in", [D, DIN])
    bmg_d = K.din("b_mgates", [1, 16])
    mng_d = K.din("mlstm_norm_g", [1, 512])
    qg_d = K.din("attn_q_norm_g", [1, 64])
    kg_d = K.din("attn_k_norm_g", [1, 64])
    sink_d = K.din("attn_sink", [1, 8])
    w_bm = K.din("w_branch_m", [512, D])
    w_ba = K.din("w_branch_a", [512, D])
    w_out = K.din("w_out", [D, D])
    w_q = K.din("peer_w_query", [D, 2048])
    skeys = K.din("peer_sub_keys", [16 * 128, 128])
    pu = K.din("peer_u", [16384, D])
    pv = K.din("peer_v", [16384, D])
    out_d = K.dout("out", [NOWN * 128, D])
    modscr = K.dscr("modscr", [2, 6 * D])
    UT_scr = K.dscr("UT_scr", [64, 128, 2 * 8 * 128], BF16)
    V_scr = K.dscr("V_scr", [64, 128, 2 * D], BF16)
    h2T_scr = K.dscr("h2T_scr", [NOWN, 128, 8 * 128], BF16)
    x1scr = K.dout("x1dbg", [NOWN * 128, D]) if dbg else K.dscr("x1scr", [NOWN * 128, D])
    if dbg:
        d_mod = K.dout("d_mod", [2, 6 * D])
        d_m = K.dout("d_m", [NOWN * 128, 512])
        d_a = K.dout("d_a", [NOWN * 128, 512])

    cst = K.sb("cst", [128, 3 * 128 + 16], F32)
    ident_f, trif_f, trir_f, iota16 = cst[:, 0:128], cst[:, 128:256], cst[:, 256:384], cst[:, 384:400]
    cstb = K.sb("cstb", [128, 3 * 128], BF16)
    ident_b, trif_b, trir_b = cstb[:, 0:128], cstb[:, 128:256], cstb[:, 256:384]
    ones_f = K.sb("ones_f", [128, 128], F32)
    amask_f = K.sb("amask_f", [128, 256], F32)
    amask_b = K.sb("amask_b", [128, 256], BF16)
    pmask = K.sb("pmask_sb", [128, NPRE * 2], F32)
    rope = K.sb("rope_sb", [128, NEXT * 64], F32)
    nmgT = K.sb("nmgT_sb", [128, 8], F32)
    featT = K.sb("featT", [128, 48 * 2], F32)
    gpT = K.sb("gpT", [128, 2 * 8], F32)
    shT = K.sb("shT", [128, 2 * 8], F32)
    bmg_bc = K.sb("bmg_bc", [128, 16], F32)
    mng_bc = K.sb("mng_bc", [128, 512], F32)
    qg_bc = K.sb("qg_bc", [128, 64], F32)
    kg_bc = K.sb("kg_bc", [128, 64], F32)
    esink = K.sb("esink", [128, 8], F32)
    epsb = K.sb("epsb", [128, 1], F32)

    DMA(lambda e: e.dma_start(out=cst[:], in_=consts_d[:, :]), w=["cst"])
    DMA(lambda e: e.dma_start(out=amask_f[:], in_=amask_d[:, :]), w=["amask_f"])
    DMA(lambda e: e.dma_start(out=pmask[:], in_=pmask_d[:, :]), w=["pmask"])
    DMA(lambda e: e.dma_start(out=rope[:], in_=rope_d[:, :]), w=["rope"])
    DMA(lambda e: e.dma_start(out=nmgT[:], in_=nmgT_d[:, :]), w=["nmgT"])
    DMA(lambda e: e.dma_start(out=bmg_bc[:], in_=bmg_d[0:1, :].partition_broadcast(128)), w=["bmg_bc"])
    DMA(lambda e: e.dma_start(out=mng_bc[:], in_=mng_d[0:1, :].partition_broadcast(128)), w=["mng_bc"])
    DMA(lambda e: e.dma_start(out=qg_bc[:], in_=qg_d[0:1, :].partition_broadcast(128)), w=["qg_bc"])
    DMA(lambda e: e.dma_start(out=kg_bc[:], in_=kg_d[0:1, :].partition_broadcast(128)), w=["kg_bc"])
    DMA(lambda e: e.dma_start(out=esink[:], in_=sink_d[0:1, :].partition_broadcast(128)), w=["esink"])
    V(lambda e: e.tensor_copy(out=cstb[:], in_=cst[:, 0:384]), r=["cst"], w=["cstb"])
    V(lambda e: e.tensor_copy(out=amask_b[:], in_=amask_f[:]), r=["amask_f"], w=["amask_b"])
    V(lambda e: e.memset(ones_f[:], 1.0), w=["ones_f"])
    V(lambda e: e.memset(epsb[:], EPS), w=["epsb"])
    A(lambda e: e.activation(out=esink[:], in_=esink[:], func=AF.Exp), r=["esink"], w=["esink"])

    def load_w_bf16(dst, dname, src, nk, c0, c1, stg_ring, dst_c0=None):
        if dst_c0 is None:
            dst_c0 = c0
        srcv = src.rearrange("(kc p) n -> p kc n", p=128)
        step = stg_ring.t[0].shape[1] // nk
        i = 0
        for cc in range(c0, c1, step):
            ce = min(cc + step, c1)
            wdt = ce - cc
            stg, sn = stg_ring.next()
            sv = stg[:, 0:nk * wdt].rearrange("p (k n) -> p k n", k=nk)
            DMA(lambda e: e.dma_start(out=sv, in_=srcv[:, :, cc:ce]), w=[sn])
            dv = dst[:, :, dst_c0 + (cc - c0):dst_c0 + (ce - c0)]
            if i % 2 == 0:
                V(lambda e: e.tensor_copy(out=dv, in_=sv), r=[sn], w=[dname])
            else:
                A(lambda e: e.copy(out=dv, in_=sv), r=[sn], w=[dname])
            i += 1

    S.set_reorder("0" in REORDER_PHASES)
    with phase_scope(K) as sc0:
        K.scope = sc0
        cT = K.sb("cT_sb", [128, 16], F32)
        scT = K.sb("scT", [128, 16], F32)
        mod_sb = K.sb("mod_sb", [2, 6 * D], F32)
        bada2 = K.sb("bada2", [2, 6 * D], F32)
        sel = K.sb("sel", [2, 128], F32)
        wa = Ring(K, "wa", [128, 8 * 512], F32, 2)
        pmod = K.ps("pmod", [128, 512], F32)
        pft = K.ps("pft", [128, 96], F32)
        DMA(lambda e: e.dma_start(out=cT[:], in_=cT_d[:, :]), w=["cT"])
        DMA(lambda e: e.dma_start(out=bada2[:], in_=b_ada[0:1, :].partition_broadcast(2)), w=["bada2"])
        A(lambda e: e.activation(out=scT[:], in_=cT[:], func=AF.Silu), r=["cT"], w=["scT"])
        scT3 = scT[:].rearrange("p (k c) -> p k c", c=2)
        wav = w_ada.rearrange("(kc p) n -> p kc n", p=128)
        for nb in range(12):
            wt, wn = wa.next()
            wv = wt[:].rearrange("p (k n) -> p k n", k=8)
            DMA(lambda e: e.dma_start(out=wv, in_=wav[:, :, nb * 512:(nb + 1) * 512]), w=[wn])
            for kc in range(8):
                T(lambda e: e.matmul(pmod[0:2, :], lhsT=scT3[:, kc, :], rhs=wv[:, kc, :], start=(kc == 0), stop=(kc == 7)),
                  r=["scT", wn], w=["pmod"])
            V(lambda e: e.tensor_tensor(out=mod_sb[:, nb * 512:(nb + 1) * 512], in0=pmod[0:2, :],
                                        in1=bada2[:, nb * 512:(nb + 1) * 512], op=ALU.add),
              r=["pmod", "bada2"], w=["mod_sb"])
        DMA(lambda e: e.dma_start(out=modscr[:, :], in_=mod_sb[:]), r=["mod_sb"], w=["modscr"])
        if dbg:
            DMA(lambda e: e.dma_start(out=d_mod[:, :], in_=mod_sb[:]), r=["mod_sb"])
        for jc in range(48):
            T(lambda e: e.transpose(out=pft[:, jc * 2:jc * 2 + 2], in_=mod_sb[0:2, jc * 128:(jc + 1) * 128],
                                    identity=ident_f[0:2, 0:2]),
              r=["mod_sb", "cst"], w=["pft"])
        V(lambda e: e.tensor_copy(out=featT[:], in_=pft[:]), r=["pft"], w=["featT"])
        f3 = featT[:].rearrange("p (j c o) -> p j c o", j=6, c=8)
        for cond in range(2):
            V(lambda e: e.scalar_tensor_tensor(out=gpT[:, cond * 8:(cond + 1) * 8], in0=f3[:, 1, :, cond], scalar=1.0,
                                               in1=nmgT[:], op0=ALU.add, op1=ALU.mult),
              r=["featT", "nmgT"], w=["gpT"])
            V(lambda e: e.tensor_copy(out=shT[:, cond * 8:(cond + 1) * 8], in_=f3[:, 0, :, cond]), r=["featT"], w=["shT"])
    K.scope = K.root
    S.barrier()

    def make_hT(K, src_rows, cond, R, ptn, pt):
        xt, xn = R["xt"].next()
        DMA(lambda e: e.dma_start(out=xt[:], in_=src_rows), w=[xn])
        ss, ssn = R["ss"].next()
        A(lambda e: e.activation(out=R["junk"][:], in_=xt[:], func=AF.Square, accum_out=ss[:, 0:1]),
          r=[xn], w=["junk", ssn])
        A(lambda e: e.activation(out=ss[:, 1:2], in_=ss[:, 0:1], func=AF.Ln, scale=1.0 / D, bias=epsb[:, 0:1]),
          r=[ssn, "epsb"], w=[ssn])
        A(lambda e: e.activation(out=ss[:, 2:3], in_=ss[:, 1:2], func=AF.Exp, scale=-0.5), r=[ssn], w=[ssn])
        xs, xsn = R["xs"].next()
        A(lambda e: e.activation(out=xs[:], in_=xt[:], func=AF.Identity, scale=ss[:, 2:3]), r=[xn, ssn], w=[xsn])
        for kc in range(8):
            T(lambda e: e.transpose(out=pt[:, kc, :], in_=xs[:, kc * 128:(kc + 1) * 128], identity=ident_b),
              r=[xsn, "cstb"], w=[ptn])
        hT, hn = R["hT"].next()
        V(lambda e: e.tensor_tensor(out=hT[:], in0=pt[:], in1=bc_mid(gpT[:, cond * 8:(cond + 1) * 8], 128), op=ALU.mult),
          r=[ptn, "gpT"], w=[hn])
        V(lambda e: e.tensor_tensor(out=hT[:], in0=hT[:], in1=bc_mid(shT[:, cond * 8:(cond + 1) * 8], 128), op=ALU.add),
          r=[hn, "shT"], w=[hn])
        return hT, hn, xt, xn

    def gate_pre(K, gps, gpsn, R):
        g, gn = R["g"].next()
        V(lambda e: e.tensor_tensor(out=g[:, 0:16], in0=gps[:, 0:16], in1=bmg_bc[:], op=ALU.add), r=[gpsn, "bmg_bc"], w=[gn])
        return {"g": g, "gn": gn}

    def gate_mid(K, G_, cps, cpsn, slot_mask=None):
        g, gn = G_["g"], G_["gn"]
        gp4 = g[:, 0:16].rearrange("p (d k h) -> p d k h", d=2, k=2)
        ef = g[:, 16:24].rearrange("p (d h) -> p d h", d=2)
        A(lambda e: e.activation(out=ef, in_=gp4[:, :, 1, :], func=AF.Exp, scale=-1.0), r=[gn], w=[gn])
        sp = g[:, 24:32]
        A(lambda e: e.activation(out=sp, in_=g[:, 16:24], func=AF.Ln, bias=1.0), r=[gn], w=[gn])
        if slot_mask is not None:
            sp3 = sp.rearrange("p (d h) -> p d h", d=2)
            V(lambda e: e.tensor_tensor(out=sp3, in0=sp3, in1=bc_mid(slot_mask, 4), op=ALU.mult), r=[gn, "pmask"], w=[gn])
        T(lambda e: e.matmul(cps[:, 0:4], lhsT=trif_f, rhs=sp[:, 0:4], start=True, stop=True), r=[gn, "cst"], w=[cpsn])
        T(lambda e: e.matmul(cps[:, 4:8], lhsT=trir_f, rhs=sp[:, 4:8], start=True, stop=True), r=[gn, "cst"], w=[cpsn])
        T(lambda e: e.matmul(cps[:, 8:16], lhsT=ones_f[:], rhs=sp, start=True, stop=True), r=[gn, "ones_f"], w=[cpsn])
        cs = g[:, 32:48]
        V(lambda e: e.tensor_copy(out=cs, in_=cps[:, 0:16]), r=[cpsn], w=[gn])

    def gate_post(K, G_, slot_mask=None):
        g, gn = G_["g"], G_["gn"]
        gp4 = g[:, 0:16].rearrange("p (d k h) -> p d k h", d=2, k=2)
        Bc, At = g[:, 32:40], g[:, 40:48]
        t1 = g[:, 48:56]
        V(lambda e: e.scalar_tensor_tensor(out=t1, in0=At, scalar=-0.5, in1=Bc, op0=ALU.mult, op1=ALU.add), r=[gn], w=[gn])
        argb = g[:, 56:64]
        V(lambda e: e.tensor_tensor(out=argb.rearrange("p (d h) -> p d h", d=2), in0=t1.rearrange("p (d h) -> p d h", d=2),
                                    in1=gp4[:, :, 0, :], op=ALU.add), r=[gn], w=[gn])
        beta = g[:, 64:72]
        A(lambda e: e.activation(out=beta, in_=argb, func=AF.Exp), r=[gn], w=[gn])
        if slot_mask is not None:
            b3 = beta.rearrange("p (d h) -> p d h", d=2)
            V(lambda e: e.scalar_tensor_tensor(out=b3, in0=b3, scalar=128.0 ** -0.5, in1=bc_mid(slot_mask, 4),
                                               op0=ALU.mult, op1=ALU.mult), r=[gn, "pmask"], w=[gn])
        else:
            V(lambda e: e.tensor_scalar(out=beta, in0=beta, scalar1=128.0 ** -0.5, scalar2=None, op0=ALU.mult), r=[gn], w=[gn])
        ea, eh, alpha = g[:, 72:80], g[:, 80:88], g[:, 88:96]
        A(lambda e: e.activation(out=ea, in_=At, func=AF.Exp, scale=-1.0), r=[gn], w=[gn])
        A(lambda e: e.activation(out=eh, in_=At, func=AF.Exp, scale=-0.5), r=[gn], w=[gn])
        A(lambda e: e.activation(out=alpha, in_=t1, func=AF.Exp, scale=-1.0), r=[gn], w=[gn])
        return gn, beta, ea, eh, alpha

    def gate_math(K, gps, gpsn, R, slot_mask=None):
        G_ = gate_pre(K, gps, gpsn, R)
        gate_mid(K, G_, gps[:, 16:32], gpsn, slot_mask)
        return gate_post(K, G_, slot_mask)

    def make_vt(K, vps, vpsn, beta, gn, R):
        res = []
        for d in range(2):
            vt, vn = R["vt%d" % d].next()
            bd = beta[:, d * 4:(d + 1) * 4]
            V(lambda e: e.tensor_tensor(out=vt[:, :, 0:128], in0=vps.rearrange("p (h v) -> p h v", h=4),
                                        in1=bc_mid(bd, 128), op=ALU.mult), r=[vpsn, gn], w=[vn])
            V(lambda e: e.tensor_copy(out=vt[:, :, 128:129], in_=bd.unsqueeze(2)), r=[gn], w=[vn])
            res.append((vt, vn))
        return res

    S.set_reorder("1" in REORDER_PHASES)
    scB = contextlib.ExitStack()
    K.scope = scB
    mT_all = K.sb("mT_all", [128, 4, NOWN * 128], BF16)
    aT_all = K.sb("aT_all", [128, 4, NOWN * 128], BF16)
    scA = contextlib.ExitStack()
    K.scope = scA
    snapF = K.sb("snapF", [128, NOWN * 4 * 129], BF16)
    snapR = K.sb("snapR", [128, NOWN * 4 * 129], BF16)
    snapF4 = snapF[:].rearrange("p (c h n) -> p c h n", c=NOWN, h=4)
    snapR4 = snapR[:].rearrange("p (c h n) -> p c h n", c=NOWN, h=4)
    with phase_scope(K) as sc1:
        K.scope = sc1
        wkv = K.sb("wkv", [128, 8, 1040], BF16)
        stg = Ring(K, "stg", [128, 1024], F32, 2)
        load_w_bf16(wkv, "wkv", w_in, 8, C_KM, C_KM + 1024, stg, dst_c0=0)
        load_w_bf16(wkv, "wkv", w_in, 8, C_G, C_G + 16, stg, dst_c0=1024)
        R = {"xt": Ring(K, "xt", [128, D], F32, 3), "ss": Ring(K, "ss", [128, 4], F32, 3),
             "xs": Ring(K, "xs", [128, D], BF16, 2), "hT": Ring(K, "hT", [128, 8, 128], BF16, 2),
             "g": Ring(K, "g", [128, 96], F32, 4), "vt0": Ring(K, "vt0_", [128, 4, 129], BF16, 2),
             "vt1": Ring(K, "vt1_", [128, 4, 129], BF16, 2), "junk": K.sb("junk", [128, D], BF16)}
        ksb = Ring(K, "ksb", [128, 512], BF16, 4)
        tmpu = Ring(K, "tmpu", [128, 4, 129], F32, 2)
        Cst = [K.sb("Cst%d" % d, [128, 4, 129], F32) for d in range(2)]
        contribR = K.sb("contribR", [128, NOWN * 4 * 129], F32)
        contribR4 = contribR[:].rearrange("p (c h n) -> p c h n", c=NOWN, h=4)
        eaR = K.sb("eaR", [128, NOWN * 4], F32)
        ehR = K.sb("ehR", [128, NOWN * 4], F32)
        pt = K.ps("pt1", [128, 8, 128], BF16)
        pk = K.ps("pk1", [128, 512], F32)
        pv_ = K.ps("pv1", [128, 512], F32)
        pg = K.ps("pg1", [128, 512], F32)
        pc = K.ps("pc1", [128, 8, 256], F32)
        for d in range(2):
            V(lambda e: e.memset(Cst[d][:], 0.0), w=["Cst%d" % d])
        pm3 = pmask[:].rearrange("p (s d) -> p s d", d=2)
        ub = Ring(K, "ub", [128, 2, D], BF16, 2)
        vb = Ring(K, "vb", [128, 2, D], BF16, 2)
        utb = Ring(K, "utb", [128, 2, 8, 128], BF16, 2)
        puv = pu.rearrange("(g c p) d -> g p c d", c=2, p=128)
        pvv = pv.rearrange("(g c p) d -> g p c d", c=2, p=128)
        conv_state = {}

        def conv_in(gI):
            vt_, vtn = vb.next()
            DMA(lambda e: e.dma_start(out=vt_[:], in_=pvv[gI]), w=[vtn], q="pool")
            ut_, utn = ub.next()
            DMA(lambda e: e.dma_start(out=ut_[:], in_=puv[gI]), w=[utn], q="pool")
            uo, uon = utb.next()
            for ci in range(2):
                for kc in range(8):
                    T(lambda e: e.transpose(out=pt[:, kc, :], in_=ut_[:, ci, kc * 128:(kc + 1) * 128], identity=ident_b),
                      r=[utn, "cstb"], w=["pt1"])
                A(lambda e: e.copy(out=uo[:, ci], in_=pt[:]), r=["pt1"], w=[uon])
            conv_state[gI] = (vt_, vtn, uo, uon)

        def conv_out(gI):
            vt_, vtn, uo, uon = conv_state.pop(gI)
            DMA(lambda e: e.dma_start(out=V_scr[gI].rearrange("p (c d) -> p c d", c=2), in_=vt_[:]), r=[vtn], w=["V_scr"], q="pool")
            DMA(lambda e: e.dma_start(out=UT_scr[gI].rearrange("p (c k e) -> p c k e", c=2, k=8), in_=uo[:]), r=[uon], w=["UT_scr"], q="pool")
        vsb = Ring(K, "vsb", [128, 512], BF16, 4)
        tiles = {}

        def slot_info(slot):
            own = slot >= NPRE
            c = slot - NPRE
            if own:
                return own, c, xown[(c + 1) * 128:(c + 2) * 128, :], 0, None
            return own, c, xpre[slot * 128:(slot + 1) * 128, :], (1 if slot < 4 else 0), pm3[:, slot, :]

        def st1(slot):
            own, c, rows, cond, smask = slot_info(slot)
            hT, hn, xt, xn = make_hT(K, rows, cond, R, "pt1", pt)
            if slot < 64:
                conv_in(slot)
            if 1 <= slot <= 64:
                conv_out(slot - 1)
            for kc in range(8):
                T(lambda e: e.matmul(pk[:], lhsT=hT[:, kc, :], rhs=wkv[:, kc, 0:512], start=(kc == 0), stop=(kc == 7)),
                  r=[hn, "wkv"], w=["pk1"])
            for kc in range(8):
                T(lambda e: e.matmul(pv_[:], lhsT=hT[:, kc, :], rhs=wkv[:, kc, 512:1024], start=(kc == 0), stop=(kc == 7)),
                  r=[hn, "wkv"], w=["pv1"])
            for kc in range(8):
                T(lambda e: e.matmul(pg[:, 0:16], lhsT=hT[:, kc, :], rhs=wkv[:, kc, 1024:1040], start=(kc == 0), stop=(kc == 7)),
                  r=[hn, "wkv"], w=["pg1a"])
            kt, kn = ksb.next()
            A(lambda e: e.copy(out=kt[:], in_=pk[:]), r=["pk1"], w=[kn])
            vt_, vtn = vsb.next()
            A(lambda e: e.copy(out=vt_[:], in_=pv_[:]), r=["pv1"], w=[vtn])
            G_ = gate_pre(K, pg, "pg1a", R)
            tiles[slot] = {"kt": kt, "kn": kn, "v": vt_, "vn": vtn, "G": G_}

        def st2(slot):
            own, c, rows, cond, smask = slot_info(slot)
            gate_mid(K, tiles[slot]["G"], pg[:, 16:32], "pg1b", smask)

        def st3(slot):
            own, c, rows, cond, smask = slot_info(slot)
            tl = tiles.pop(slot)
            kt, kn = tl["kt"], tl["kn"]
            gn, beta, ea, eh, alpha = gate_post(K, tl["G"], smask)
            vts = make_vt(K, tl["v"][:], tl["vn"], beta, gn, R)
            for d in range(2):
                vt, vn = vts[d]
                for h in range(4):
                    T(lambda e: e.matmul(pc[:, d * 4 + h, 0:129], lhsT=kt[:, h * 128:(h + 1) * 128], rhs=vt[:, h, :],
                                         start=True, stop=True), r=[kn, vn], w=["pc1_%d" % d])
            for d in range(2):
                ead, ehd = ea[:, d * 4:(d + 1) * 4], eh[:, d * 4:(d + 1) * 4]
                cn = "Cst%d" % d
                if own and d == 0:
                    V(lambda e: e.tensor_tensor(out=snapF4[:, c], in0=Cst[0][:], in1=bc_mid(ehd, 129), op=ALU.mult),
                      r=[cn, gn], w=["snapF"])
                if own and d == 1:
                    V(lambda e: e.tensor_tensor(out=contribR4[:, c], in0=pc[:, 4:8, 0:129], in1=bc_mid(ehd, 129), op=ALU.mult),
                      r=["pc1_1", gn], w=["contribR"])
                    V(lambda e: e.tensor_copy(out=eaR[:, c * 4:(c + 1) * 4], in_=ead), r=[gn], w=["eaR"])
                    V(lambda e: e.tensor_copy(out=ehR[:, c * 4:(c + 1) * 4], in_=ehd), r=[gn], w=["ehR"])
                    continue
                tu, tn = tmpu.next()
                V(lambda e: e.tensor_tensor(out=tu[:], in0=pc[:, d * 4:(d + 1) * 4, 0:129], in1=bc_mid(ehd, 129), op=ALU.mult),
                  r=["pc1_%d" % d, gn], w=[tn])
                V(lambda e: e.tensor_tensor(out=Cst[d][:], in0=Cst[d][:], in1=bc_mid(ead, 129), op=ALU.mult), r=[cn, gn], w=[cn])
                V(lambda e: e.tensor_tensor(out=Cst[d][:], in0=Cst[d][:], in1=tu[:], op=ALU.add), r=[cn, tn], w=[cn])

        NS = NPRE + NOWN
        for it in range(NS + 2):
            if it < NS:
                st1(it)
            if 0 <= it - 2 < NS:
                st3(it - 2)
            if 0 <= it - 1 < NS:
                st2(it - 1)
        for c in range(NOWN - 1, -1, -1):
            ead, ehd = eaR[:, c * 4:(c + 1) * 4], ehR[:, c * 4:(c + 1) * 4]
            V(lambda e: e.tensor_tensor(out=snapR4[:, c], in0=Cst[1][:], in1=bc_mid(ehd, 129), op=ALU.mult),
              r=["Cst1", "ehR"], w=["snapR"])
            V(lambda e: e.tensor_tensor(out=Cst[1][:], in0=Cst[1][:], in1=bc_mid(ead, 129), op=ALU.mult), r=["Cst1", "eaR"], w=["Cst1"])
            V(lambda e: e.tensor_tensor(out=Cst[1][:], in0=Cst[1][:], in1=contribR4[:, c], op=ALU.add), r=["Cst1", "contribR"], w=["Cst1"])
    K.scope = K.root
    S.barrier()

    S.set_reorder("2a" in REORDER_PHASES)
    with phase_scope(K) as sc2:
        K.scope = sc2
        NA = C_MG
        wA = K.sb("wA", [128, 8, NA], BF16)
        stg = Ring(K, "stg2", [128, 1024], F32, 2)
        load_w_bf16(wA, "wA", w_in, 8, 0, NA, stg)
        R = {"xt": Ring(K, "xt2", [128, D], F32, 2), "ss": Ring(K, "ss2", [128, 4], F32, 3),
             "xs": Ring(K, "xs2", [128, D], BF16, 2), "hT": Ring(K, "hT2", [128, 8, 128], BF16, 2),
             "g": Ring(K, "g2", [128, 96], F32, 2), "vt0": Ring(K, "vt02_", [128, 4, 129], BF16, 2),
             "vt1": Ring(K, "vt12_", [128, 4, 129], BF16, 2), "junk": K.sb("junk2", [128, D], BF16)}
        kT_all = K.sb("kT_all", [128, 20, 128], BF16)
        v_all = K.sb("v_all", [128, 20, 2, 65], BF16)
        qTa = Ring(K, "qTa", [128, 4, 128], BF16, 3)
        qTm = Ring(K, "qTm", [128, 4, 128], BF16, 2)
        kTm = Ring(K, "kTm", [128, 4, 128], BF16, 2)
        og = Ring(K, "og", [128, 512], F32, 2)
        smf = Ring(K, "smf", [128, 4, 128], BF16, 2)
        smr = Ring(K, "smr", [128, 4, 128], BF16, 2)
        pr = Ring(K, "pr", [128, 4, 128], BF16, 10)
        wk = Ring(K, "wk", [128, 512], F32, 6)
        sm = Ring(K, "sm", [128, 32], F32, 6)
        mb = Ring(K, "mb", [128, 512], BF16, 3)
        pt = K.ps("pt2", [128, 8, 128], BF16)
        pab = K.ps("pab2", [128, 1024], F32)
        pn = K.ps("pn2", [128, 8, 256], F32)
        pcs = K.ps("pcs2", [128, 512], F32)
        pabi = [0]

        def proj(hT, hn, c0, width, transposed=False):
            i = pabi[0] % 2
            pabi[0] += 1
            nm = "pab2_%d" % i
            if not transposed:
                outp = pab[:, i * 512:i * 512 + width]
                for kc in range(8):
                    T(lambda e: e.matmul(outp, lhsT=hT[:, kc, :], rhs=wA[:, kc, c0:c0 + width], start=(kc == 0), stop=(kc == 7)),
                      r=[hn, "wA"], w=[nm])
            else:
                outp = pab[:, i * 512:(i + 1) * 512]
                for hh in range(width // 128):
                    for kc in range(8):
                        T(lambda e: e.matmul(outp[:, hh * 128:(hh + 1) * 128], lhsT=wA[:, kc, c0 + hh * 128:c0 + (hh + 1) * 128],
                                             rhs=hT[:, kc, :], start=(kc == 0), stop=(kc == 7)), r=[hn, "wA"], w=[nm])
            return outp, nm

        V(lambda e: e.memset(v_all[:], 1.0), w=["v_all"])

        def qk_norm_rope(src, srcn, G_, J_, gbc, gbn, e_idx, out_ap, outn):
            nh = G_ * J_
            sq, sqn = wk.next()
            A(lambda e: e.activation(out=sq[:, 0:nh * 64], in_=src, func=AF.Square), r=[srcn], w=[sqn])
            st_, stn = sm.next()
            V(lambda e: e.tensor_reduce(out=st_[:, 0:nh], in_=sq[:, 0:nh * 64].rearrange("p (h d) -> p h d", h=nh),
                                        axis=AX.X, op=ALU.add), r=[sqn], w=[stn])
            A(lambda e: e.activation(out=st_[:, 8:8 + nh], in_=st_[:, 0:nh], func=AF.Ln, scale=1.0 / 64, bias=epsb[:, 0:1]),
              r=[stn, "epsb"], w=[stn])
            A(lambda e: e.activation(out=st_[:, 16:16 + nh], in_=st_[:, 8:8 + nh], func=AF.Exp, scale=-0.5), r=[stn], w=[stn])
            qn, qnn = wk.next()
            qn3 = qn[:, 0:nh * 64].rearrange("p (h d) -> p h d", h=nh)
            V(lambda e: e.tensor_tensor(out=qn3, in0=src.rearrange("p (h d) -> p h d", h=nh), in1=bc_mid(st_[:, 16:16 + nh], 64),
                                        op=ALU.mult), r=[srcn, stn], w=[qnn])
            V(lambda e: e.tensor_tensor(out=qn3, in0=qn3, in1=gbc[:].unsqueeze(1).to_broadcast([128, nh, 64]), op=ALU.mult),
              r=[qnn, gbn], w=[qnn])
            qn4 = qn[:, 0:nh * 64].rearrange("p (g j d) -> p g j d", g=G_, j=J_)
            o4 = out_ap.rearrange("p (j g d) -> p g j d", j=J_, g=G_)
            if e_idx is None:
                V(lambda e: e.tensor_copy(out=o4, in_=qn4), r=[qnn], w=[outn])
                return
            cos = rope[:, e_idx * 64:e_idx * 64 + 32].unsqueeze(1).to_broadcast([128, nh, 32])
            sin = rope[:, e_idx * 64 + 32:e_idx * 64 + 64].unsqueeze(1).to_broadcast([128, nh, 32])
            tw, twn = wk.next()
            tA = tw[:, 0:nh * 32].rearrange("p (h d) -> p h d", h=nh)
            tB = tw[:, 256:256 + nh * 32].rearrange("p (h d) -> p h d", h=nh)
            q1, q2 = qn3[:, :, 0:32], qn3[:, :, 32:64]
            V(lambda e: e.tensor_tensor(out=tA, in0=q1, in1=cos, op=ALU.mult), r=[qnn, "rope"], w=[twn])
            V(lambda e: e.tensor_tensor(out=tB, in0=q2, in1=sin, op=ALU.mult), r=[qnn, "rope"], w=[twn])
            tA4 = tw[:, 0:nh * 32].rearrange("p (g j d) -> p g j d", g=G_, j=J_)
            tB4 = tw[:, 256:256 + nh * 32].rearrange("p (g j d) -> p g j d", g=G_, j=J_)
            V(lambda e: e.tensor_tensor(out=o4[:, :, :, 0:32], in0=tA4, in1=tB4, op=ALU.subtract), r=[twn], w=[outn])
            tw2, twn2 = wk.next()
            tC = tw2[:, 0:nh * 32].rearrange("p (h d) -> p h d", h=nh)
            tD = tw2[:, 256:256 + nh * 32].rearrange("p (h d) -> p h d", h=nh)
            V(lambda e: e.tensor_tensor(out=tC, in0=q2, in1=cos, op=ALU.mult), r=[qnn, "rope"], w=[twn2])
            V(lambda e: e.tensor_tensor(out=tD, in0=q1, in1=sin, op=ALU.mult), r=[qnn, "rope"], w=[twn2])
            tC4 = tw2[:, 0:nh * 32].rearrange("p (g j d) -> p g j d", g=G_, j=J_)
            tD4 = tw2[:, 256:256 + nh * 32].rearrange("p (g j d) -> p g j d", g=G_, j=J_)
            V(lambda e: e.tensor_tensor(out=o4[:, :, :, 32:64], in0=tC4, in1=tD4, op=ALU.add), r=[twn2], w=[outn])

        def attn_kv(hT, hn, e_store, e_rope):
            ps_, pn_ = proj(hT, hn, C_AK, 256)
            kr, krn = mb.next()
            qk_norm_rope(ps_[:, 0:128], pn_, 2, 1, kg_bc, "kg_bc", e_rope, kr[:, 0:128], krn)
            V(lambda e: e.tensor_copy(out=v_all[:, e_store, :, 0:64], in_=ps_[:, 128:256].rearrange("p (h d) -> p h d", h=2)),
              r=[pn_], w=["v_all"])
            T(lambda e: e.transpose(out=pt[:, 0, :], in_=kr[:, 0:128], identity=ident_b), r=[krn, "cstb"], w=["pt2"])
            V(lambda e: e.tensor_copy(out=kT_all[:, e_store, :], in_=pt[:, 0, :]), r=["pt2"], w=["kT_all"])

        def own_part(c, hT, hn, e_idx):
            gps, gpn = proj(hT, hn, C_G, 16)
            gfull = pab[:, (pabi[0] - 1) % 2 * 512:((pabi[0] - 1) % 2 + 1) * 512]
            gn, beta, ea, eh, alpha = gate_math(K, gfull, gpn, R, None)
            vps, vpn = proj(hT, hn, C_VM, 512)
            vts = make_vt(K, vps, vpn, beta, gn, R)
            qps, qpn = proj(hT, hn, C_QM, 512, transposed=True)
            qt, qtn = qTm.next()
            A(lambda e: e.copy(out=qt[:], in_=qps.rearrange("p (h t) -> p h t", h=4)), r=[qpn], w=[qtn])
            kps, kpn = proj(hT, hn, C_KM, 512, transposed=True)
            ktm, ktn = kTm.next()
            A(lambda e: e.copy(out=ktm[:], in_=kps.rearrange("p (h t) -> p h t", h=4)), r=[kpn], w=[ktn])
            ops, opn = proj(hT, hn, C_OM, 512)
            ogt, ogn = og.next()
            A(lambda e: e.activation(out=ogt[:], in_=ops, func=AF.Sigmoid), r=[opn], w=[ogn])
            V(lambda e: e.tensor_tensor(out=ogt[:], in0=ogt[:], in1=mng_bc[:], op=ALU.mult), r=[ogn, "mng_bc"], w=[ogn])
            for h in range(4):
                T(lambda e: e.matmul(pcs[:, h * 128:(h + 1) * 128], lhsT=ktm[:, h, :], rhs=qt[:, h, :], start=True, stop=True),
                  r=[ktn, qtn], w=["pcs2"])
            sf, sfn = smf.next()
            sr, srn = smr.next()
            pcs3 = pcs[:].rearrange("p (h t) -> p h t", h=4)
            V(lambda e: e.tensor_tensor(out=sf[:], in0=pcs3, in1=trif_f.unsqueeze(1).to_broadcast([128, 4, 128]), op=ALU.mult),
              r=["pcs2", "cst"], w=[sfn])
            V(lambda e: e.tensor_tensor(out=sr[:], in0=pcs3, in1=trir_f.unsqueeze(1).to_broadcast([128, 4, 128]), op=ALU.mult),
              r=["pcs2", "cst"], w=[srn])
            yield
            for d in range(2):
                smt, smn = (sf, sfn) if d == 0 else (sr, srn)
                vt, vn = vts[d]
                snap4, snn = (snapF4, "snapF") if d == 0 else (snapR4, "snapR")
                for h in range(4):
                    T(lambda e: e.matmul(pn[:, d * 4 + h, 0:129], lhsT=smt[:, h, :], rhs=vt[:, h, :], start=True, stop=False),
                      r=[smn, vn], w=["pn2"])
                    T(lambda e: e.matmul(pn[:, d * 4 + h, 0:129], lhsT=qt[:, h, :], rhs=snap4[:, c, h, :], start=False, stop=True),
                      r=[qtn, snn], w=["pn2"])
            st_, stn = sm.next()
            V(lambda e: e.tensor_tensor(out=st_[:, 0:8], in0=pn[:, :, 128], in1=alpha, op=ALU.mult), r=["pn2", gn], w=[stn])
            V(lambda e: e.tensor_scalar(out=st_[:, 16:24], in0=st_[:, 0:8], scalar1=1.0, scalar2=None, op0=ALU.max), r=[stn], w=[stn])
            V(lambda e: e.scalar_tensor_tensor(out=st_[:, 8:16], in0=st_[:, 0:8], scalar=-1.0, in1=st_[:, 16:24], op0=ALU.mult, op1=ALU.max),
              r=[stn], w=[stn])
            V(lambda e: e.reciprocal(out=st_[:, 16:24], in_=st_[:, 8:16]), r=[stn], w=[stn])
            V(lambda e: e.tensor_tensor(out=st_[:, 24:32], in0=st_[:, 16:24], in1=alpha, op=ALU.mult), r=[stn, gn], w=[stn])
            h0, h0n = wk.next()
            h1, h1n = wk.next()
            h03 = h0[:].rearrange("p (h v) -> p h v", h=4)
            h13 = h1[:].rearrange("p (h v) -> p h v", h=4)
            V(lambda e: e.tensor_tensor(out=h03, in0=pn[:, 0:4, 0:128], in1=bc_mid(st_[:, 24:28], 128), op=ALU.mult),
              r=["pn2", stn], w=[h0n])
            V(lambda e: e.tensor_tensor(out=h13, in0=pn[:, 4:8, 0:128], in1=bc_mid(st_[:, 28:32], 128), op=ALU.mult),
              r=["pn2", stn], w=[h1n])
            V(lambda e: e.tensor_tensor(out=h0[:], in0=h0[:], in1=h1[:], op=ALU.add), r=[h0n, h1n], w=[h0n])
            A(lambda e: e.activation(out=h1[:], in_=h0[:], func=AF.Square), r=[h0n], w=[h1n])
            s2, s2n = sm.next()
            V(lambda e: e.tensor_reduce(out=s2[:, 0:4], in_=h13, axis=AX.X, op=ALU.add), r=[h1n], w=[s2n])
            A(lambda e: e.activation(out=s2[:, 4:8], in_=s2[:, 0:4], func=AF.Ln, scale=1.0 / 128, bias=epsb[:, 0:1]),
              r=[s2n, "epsb"], w=[s2n])
            A(lambda e: e.activation(out=s2[:, 8:12], in_=s2[:, 4:8], func=AF.Exp, scale=-0.5), r=[s2n], w=[s2n])
            V(lambda e: e.tensor_tensor(out=h03, in0=h03, in1=bc_mid(s2[:, 8:12], 128), op=ALU.mult), r=[h0n, s2n], w=[h0n])
            mt, mtn = mb.next()
            V(lambda e: e.tensor_tensor(out=mt[:], in0=h0[:], in1=ogt[:], op=ALU.mult), r=[h0n, ogn], w=[mtn])
            if dbg:
                DMA(lambda e: e.dma_start(out=d_m[c * 128:(c + 1) * 128, :], in_=h0[:]), r=[h0n])
            for kc in range(4):
                T(lambda e: e.transpose(out=pt[:, kc, :], in_=mt[:, kc * 128:(kc + 1) * 128], identity=ident_b),
                  r=[mtn, "cstb"], w=["pt2"])
            V(lambda e: e.tensor_copy(out=mT_all[:, :, c * 128:(c + 1) * 128], in_=pt[:, 0:4, :]), r=["pt2"], w=["mT_all"])
            aps, apn = proj(hT, hn, C_AQ, 512)
            qr, qrn = mb.next()
            qk_norm_rope(aps, apn, 2, 4, qg_bc, "qg_bc", e_idx, qr[:], qrn)
            for j in range(4):
                T(lambda e: e.transpose(out=pt[:, 4 + j, :], in_=qr[:, j * 128:(j + 1) * 128], identity=ident_b),
                  r=[qrn, "cstb"], w=["pt2"])
            qa, qan = qTa.next()
            V(lambda e: e.tensor_copy(out=qa[:], in_=pt[:, 4:8, :]), r=["pt2"], w=[qan])
            qas[c] = (qa, qan)

        def attention(c, qa, qan):
            kts = [c, c + 1, c + 2, 18, 19]
            ps_list = {}
            for g in range(2):
                for ki, kt in enumerate(kts):
                    i = pabi[0] % 2
                    pabi[0] += 1
                    nm = "pab2_%d" % i
                    outp = pab[:, i * 512:(i + 1) * 512]
                    T(lambda e: e.matmul(outp, lhsT=kT_all[g * 64:(g + 1) * 64, kt, :],
                                         rhs=qa[g * 64:(g + 1) * 64, :, :].rearrange("p j t -> p (j t)"), start=True, stop=True),
                      r=["kT_all", qan], w=[nm])
                    p_, pn_ = pr.next()
                    A(lambda e: e.activation(out=p_[:].rearrange("p j t -> p (j t)"), in_=outp, func=AF.Exp, scale=0.125),
                      r=[nm], w=[pn_])
                    if ki == 0 or ki == 2:
                        if ki == 0:
                            mk = amask_b[:, 0:128] if c == 0 else trir_b
                        else:
                            mk = amask_b[:, 128:256] if c == NOWN - 1 else trif_b
                        V(lambda e: e.tensor_tensor(out=p_[:], in0=p_[:], in1=mk.unsqueeze(1).to_broadcast([128, 4, 128]),
                                                    op=ALU.mult), r=[pn_, "cstb", "amask_b"], w=[pn_])
                    ps_list[(g, ki)] = (p_, pn_)
            yield
            for g in range(2):
                for j in range(4):
                    hq = g * 4 + j
                    for ki, kt in enumerate(kts):
                        p_, pn_ = ps_list[(g, ki)]
                        T(lambda e: e.matmul(pn[:, hq, 0:65], lhsT=p_[:, j, :], rhs=v_all[:, kt, g, :], start=(ki == 0), stop=(ki == 4)),
                          r=[pn_, "v_all"], w=["pn2"])
            st_, stn = sm.next()
            V(lambda e: e.tensor_tensor(out=st_[:, 0:8], in0=pn[:, :, 64], in1=esink[:], op=ALU.add), r=["pn2", "esink"], w=[stn])
            V(lambda e: e.reciprocal(out=st_[:, 8:16], in_=st_[:, 0:8]), r=[stn], w=[stn])
            at, atn = mb.next()
            V(lambda e: e.tensor_tensor(out=at[:].rearrange("p (h d) -> p h d", h=8), in0=pn[:, :, 0:64], in1=bc_mid(st_[:, 8:16], 64),
                                        op=ALU.mult), r=["pn2", stn], w=[atn])
            if dbg:
                DMA(lambda e: e.dma_start(out=d_a[c * 128:(c + 1) * 128, :], in_=at[:]), r=[atn], q="pool")
            for kc in range(4):
                T(lambda e: e.transpose(out=pt[:, kc, :], in_=at[:, kc * 128:(kc + 1) * 128], identity=ident_b),
                  r=[atn, "cstb"], w=["pt2"])
            V(lambda e: e.tensor_copy(out=aT_all[:, :, c * 128:(c + 1) * 128], in_=pt[:, 0:4, :]), r=["pt2"], w=["aT_all"])

        for i in range(2):
            hT, hn, xt, xn = make_hT(K, xpre[i * 128:(i + 1) * 128, :], 1, R, "pt2", pt)
            attn_kv(hT, hn, 18 + i, None)
        qas = {}
        fronts = {}

        def front(e_idx):
            hT, hn, xt, xn = make_hT(K, xown[e_idx * 128:(e_idx + 1) * 128, :], 0, R, "pt2", pt)
            attn_kv(hT, hn, e_idx, e_idx)
            fronts[e_idx] = (hT, hn)

        front(0)
        for e_idx in range(NEXT):
            hT, hn = fronts.pop(e_idx)
            go = own_part(e_idx - 1, hT, hn, e_idx) if 1 <= e_idx <= NOWN else None
            ga = None
            if e_idx >= 2:
                qa, qan = qas.pop(e_idx - 2)
                ga = attention(e_idx - 2, qa, qan)
            if go is not None:
                next(go)
            if ga is not None:
                next(ga)
            if go is not None:
                for _ in go:
                    pass
            if e_idx + 1 < NEXT:
                front(e_idx + 1)
            if ga is not None:
                for _ in ga:
                    pass
    S.flush()
    scA.close()
    K.scope = K.root
    S.barrier()

    S.set_reorder("2b" in REORDER_PHASES)
    with phase_scope(K) as sc3:
        K.scope = sc3
        wG = K.sb("wG", [128, 8, 2048], BF16)
        wbm = K.sb("wbm", [128, 4, D], BF16)
        wba = K.sb("wba", [128, 4, D], BF16)
        wo = K.sb("wo", [128, 8, D], BF16)
        stg = Ring(K, "stg3", [128, 2048], F32, 2)
        load_w_bf16(wG, "wG", w_in, 8, C_MG, DIN, stg, dst_c0=0)
        load_w_bf16(wbm, "wbm", w_bm, 4, 0, D, stg)
        load_w_bf16(wba, "wba", w_ba, 4, 0, D, stg)
        load_w_bf16(wo, "wo", w_out, 8, 0, D, stg)
        g1bc = K.sb("g1bc", [128, D], F32)
        DMA(lambda e: e.dma_start(out=g1bc[:], in_=modscr[0:1, 2 * D:3 * D].partition_broadcast(128)), r=["modscr"], w=["g1bc"])
        R = {"xt": Ring(K, "xt3", [128, D], F32, 3), "ss": Ring(K, "ss3", [128, 4], F32, 3),
             "xs": Ring(K, "xs3", [128, D], BF16, 2), "hT": Ring(K, "hT3", [128, 8, 128], BF16, 2),
             "junk": K.sb("junk3", [128, D], BF16)}
        sg = Ring(K, "sg", [128, 2048], BF16, 2)
        t1r = Ring(K, "t1r", [128, D], F32, 2)
        zb = Ring(K, "zb", [128, D], BF16, 2)
        zT = Ring(K, "zT", [128, 8, 128], BF16, 2)
        x1r = Ring(K, "x1r", [128, D], F32, 2)
        pt = K.ps("pt3", [128, 8, 128], BF16)
        pab = K.ps("pab3", [128, 1024], F32)
        pn = K.ps("pn3", [128, 2048], F32)
        st2b = {}

        def stA(c):
            hT, hn, xt, xn = make_hT(K, xown[(c + 1) * 128:(c + 2) * 128, :], 0, R, "pt3", pt)
            sgt, sgn = sg.next()
            for q in range(4):
                i = q % 2
                nm = "pab3_%d" % i
                outp = pab[:, i * 512:(i + 1) * 512]
                for kc in range(8):
                    T(lambda e: e.matmul(outp, lhsT=hT[:, kc, :], rhs=wG[:, kc, q * 512:(q + 1) * 512], start=(kc == 0), stop=(kc == 7)),
                      r=[hn, "wG"], w=[nm])
                A(lambda e: e.activation(out=sgt[:, q * 512:(q + 1) * 512], in_=outp, func=AF.Sigmoid), r=[nm], w=[sgn])
            st2b[c] = (sgt, sgn, xt, xn)

        def stB(c):
            sgt, sgn, xt, xn = st2b.pop(c)
            for half in range(2):
                for kc in range(4):
                    T(lambda e: e.matmul(pn[:, half * 512:(half + 1) * 512], lhsT=mT_all[:, kc, c * 128:(c + 1) * 128],
                                         rhs=wbm[:, kc, half * 512:(half + 1) * 512], start=(kc == 0), stop=(kc == 3)),
                      r=["mT_all", "wbm"], w=["pn3_m"])
            for half in range(2):
                for kc in range(4):
                    T(lambda e: e.matmul(pn[:, 1024 + half * 512:1024 + (half + 1) * 512], lhsT=aT_all[:, kc, c * 128:(c + 1) * 128],
                                         rhs=wba[:, kc, half * 512:(half + 1) * 512], start=(kc == 0), stop=(kc == 3)),
                      r=["aT_all", "wba"], w=["pn3_a"])
            t1, t1n = t1r.next()
            V(lambda e: e.tensor_tensor(out=t1[:], in0=pn[:, 0:1024], in1=sgt[:, 0:1024], op=ALU.mult), r=["pn3_m", sgn], w=[t1n])
            t2, t2n = t1r.next()
            V(lambda e: e.tensor_tensor(out=t2[:], in0=pn[:, 1024:2048], in1=sgt[:, 1024:2048], op=ALU.mult), r=["pn3_a", sgn], w=[t2n])
            z, zn = zb.next()
            V(lambda e: e.tensor_tensor(out=z[:], in0=t1[:], in1=t2[:], op=ALU.add), r=[t1n, t2n], w=[zn])
            for kc in range(8):
                T(lambda e: e.transpose(out=pt[:, kc, :], in_=z[:, kc * 128:(kc + 1) * 128], identity=ident_b), r=[zn, "cstb"], w=["pt3"])
            zt, ztn = zT.next()
            A(lambda e: e.copy(out=zt[:], in_=pt[:]), r=["pt3"], w=[ztn])
            for half in range(2):
                for kc in range(8):
                    T(lambda e: e.matmul(pn[:, half * 512:(half + 1) * 512], lhsT=zt[:, kc, :], rhs=wo[:, kc, half * 512:(half + 1) * 512],
                                         start=(kc == 0), stop=(kc == 7)), r=[ztn, "wo"], w=["pn3_m"])
            x1, x1n = x1r.next()
            V(lambda e: e.tensor_tensor(out=x1[:], in0=pn[:, 0:1024], in1=g1bc[:], op=ALU.mult), r=["pn3_m", "g1bc"], w=[x1n])
            V(lambda e: e.tensor_tensor(out=x1[:], in0=x1[:], in1=xt[:], op=ALU.add), r=[x1n, xn], w=[x1n])
            DMA(lambda e: e.dma_start(out=x1scr[c * 128:(c + 1) * 128, :], in_=x1[:]), r=[x1n], w=["x1scr"])

        stA(0)
        for c in range(NOWN):
            if c + 1 < NOWN:
                stA(c + 1)
            stB(c)
    S.flush()
    scB.close()
    K.scope = K.root
    S.barrier()

    if dbg == 2:
        S.finish()
        K.root.close()
        return nc

    TB = 256
    NTB = NOWN * 128 // TB
    scP = contextlib.ExitStack()
    K.scope = scP
    i1T_all = K.sb("i1T_all", [128, NOWN * 128], BF16)
    i2T_all = K.sb("i2T_all", [128, NOWN * 128], BF16)
    gT_all = K.sb("gT_all", [128, NOWN * 128], BF16)
    iota128 = K.sb("iota128", [128, 128], F32)
    G(lambda e: e.iota(iota128[:], pattern=[[1, 128]], base=0, channel_multiplier=0, allow_small_or_imprecise_dtypes=True), w=["iota128"])

    S.set_reorder("3.1" in REORDER_PHASES)
    with phase_scope(K) as sc4:
        K.scope = sc4
        wq = K.sb("wq", [128, 8, 2048], BF16)
        stg = Ring(K, "stg4", [128, 1024], F32, 2)
        load_w_bf16(wq, "wq", w_q, 8, 0, 2048, stg)
        skT = K.sb("skT", [128, 16, 128], BF16)
        g2p = K.sb("g2p", [128, D], F32)
        sh2bc = K.sb("sh2bc", [128, D], F32)
        DMA(lambda e: e.dma_start(out=sh2bc[:], in_=modscr[0:1, 3 * D:4 * D].partition_broadcast(128)), r=["modscr"], w=["sh2bc"])
        DMA(lambda e: e.dma_start(out=g2p[:], in_=modscr[0:1, 4 * D:5 * D].partition_broadcast(128)), r=["modscr"], w=["g2p"])
        DMA(lambda e: e.dma_start(out=stg.t[0][:, 0:D], in_=nfg_d[0:1, :].partition_broadcast(128)), w=[stg.names[0]])
        V(lambda e: e.scalar_tensor_tensor(out=g2p[:], in0=g2p[:], scalar=1.0, in1=stg.t[0][:, 0:D], op0=ALU.add, op1=ALU.mult),
          r=["g2p", stg.names[0]], w=["g2p"])
        pt = K.ps("pt4", [128, 8, 128], BF16)
        pq = K.ps("pq4", [128, 2048], F32)
        ptf = K.ps("ptf4", [128, 512], F32)
        x1t = Ring(K, "x1t", [128, D], F32, 2)
        ssr = Ring(K, "ss4", [128, 4], F32, 2)
        junk = K.sb("junk4", [128, D], BF16)
        h2 = Ring(K, "h2", [128, D], F32, 2)
        h2b = Ring(K, "h2b", [128, D], BF16, 2)
        h2Tr = Ring(K, "h2Tr", [128, 8, 128], BF16, 2)
        qT = Ring(K, "qT", [128, 16, 128], BF16, 2)
        s_sbr = Ring(K, "s_sb", [128, 16, 128], F32, 2)
        sk_f, sk_b, sk_bn = s_sbr.t[0], qT.t[0], qT.names[0]
        DMA(lambda e: e.dma_start(out=sk_f[:], in_=skeys.rearrange("(hp n) d -> n hp d", n=128)), w=[s_sbr.names[0]])
        V(lambda e: e.tensor_copy(out=sk_b[:], in_=sk_f[:]), r=[s_sbr.names[0]], w=[sk_bn])
        for hp in range(16):
            T(lambda e: e.transpose(out=pt[:, hp % 8, :], in_=sk_b[:, hp, :], identity=ident_b), r=[sk_bn, "cstb"], w=["pt4"])
            V(lambda e: e.tensor_copy(out=skT[:, hp, :], in_=pt[:, hp % 8, :]), r=["pt4"], w=["skT"])
        s_wk = K.sb("s_wk", [128, 16, 128], F32)
        cwk = s_wk[:].rearrange("p a b -> p (a b)").rearrange("p (h c) -> p h c", h=8)
        tv = K.sb("tv", [128, 16, 16], F32)
        tix = K.sb("tix", [128, 16, 16], U32)
        tixf = K.sb("tixf", [128, 16, 16], F32)
        cand = K.sb("cand", [128, 8, 256], F32)
        cv = K.sb("cv", [128, 8, 16], F32)
        cpos = K.sb("cpos", [128, 8, 16], U32)
        cj = K.sb("cj", [128, 2, 128], U32)
        cjf = K.sb("cjf", [128, 2, 128], F32)
        oh = K.sb("oh", [128, 128, 16], F32)
        oh2 = K.sb("oh2", [128, 128, 16], F32)
        iif = K.sb("iif", [128, 2, 128], F32)
        gw = K.sb("gw", [128, 8, 16], F32)
        gst = K.sb("gst", [128, 32], F32)
        NEG = -3.0e38
        def route_tile(it):
            xt, xn = x1t.next()
            DMA(lambda e: e.dma_start(out=xt[:], in_=x1scr[it * 128:(it + 1) * 128, :]), r=["x1scr"], w=[xn])
            ss, ssn = ssr.next()
            A(lambda e: e.activation(out=junk[:], in_=xt[:], func=AF.Square, accum_out=ss[:, 0:1]), r=[xn], w=["junk4", ssn])
            A(lambda e: e.activation(out=ss[:, 1:2], in_=ss[:, 0:1], func=AF.Ln, scale=1.0 / D, bias=epsb[:, 0:1]), r=[ssn, "epsb"], w=[ssn])
            A(lambda e: e.activation(out=ss[:, 2:3], in_=ss[:, 1:2], func=AF.Exp, scale=-0.5), r=[ssn], w=[ssn])
            ht, htn = h2.next()
            V(lambda e: e.scalar_tensor_tensor(out=ht[:], in0=xt[:], scalar=ss[:, 2:3], in1=g2p[:], op0=ALU.mult, op1=ALU.mult),
              r=[xn, ssn, "g2p"], w=[htn])
            hb, hbn = h2b.next()
            V(lambda e: e.tensor_tensor(out=hb[:], in0=ht[:], in1=sh2bc[:], op=ALU.add), r=[htn, "sh2bc"], w=[hbn])
            for kc in range(8):
                T(lambda e: e.transpose(out=pt[:, kc, :], in_=hb[:, kc * 128:(kc + 1) * 128], identity=ident_b), r=[hbn, "cstb"], w=["pt4"])
            hTt, hTtn = h2Tr.next()
            A(lambda e: e.copy(out=hTt[:], in_=pt[:]), r=["pt4"], w=[hTtn])
            DMA(lambda e: e.dma_start(out=h2T_scr[it].rearrange("p (k t) -> p k t", k=8), in_=hTt[:]), r=[hTtn], w=["h2T_scr"])
            for hp in range(16):
                for kc in range(8):
                    T(lambda e: e.matmul(pq[:, hp * 128:(hp + 1) * 128], lhsT=wq[:, kc, hp * 128:(hp + 1) * 128], rhs=hTt[:, kc, :],
                                         start=(kc == 0), stop=(kc == 7)), r=[hTtn, "wq"], w=["pq4"])
            qt, qtn = qT.next()
            A(lambda e: e.copy(out=qt[:], in_=pq[:].rearrange("p (a b) -> p a b", a=16)), r=["pq4"], w=[qtn])
            for hp in range(16):
                T(lambda e: e.matmul(pq[:, hp * 128:(hp + 1) * 128], lhsT=qt[:, hp, :], rhs=skT[:, hp, :], start=True, stop=True),
                  r=[qtn, "skT"], w=["pq4"])
            s_sb, s_sbn = s_sbr.next()
            A(lambda e: e.copy(out=s_sb[:], in_=pq[:].rearrange("p (a b) -> p a b", a=16)), r=["pq4"], w=[s_sbn])
            yield
            for hp in range(16):
                V(lambda e: e.max(out=tv[:, hp, 0:8], in_=s_sb[:, hp, :]), r=[s_sbn], w=["tv"])
                V(lambda e: e.max_index(out=tix[:, hp, 0:8], in_max=tv[:, hp, 0:8], in_values=s_sb[:, hp, :]), r=[s_sbn, "tv"], w=["tix"])
                V(lambda e: e.match_replace(out=s_wk[:, hp, :], in_to_replace=tv[:, hp, 0:8], in_values=s_sb[:, hp, :], imm_value=NEG),
                  r=[s_sbn, "tv"], w=["s_wk"])
                V(lambda e: e.max(out=tv[:, hp, 8:16], in_=s_wk[:, hp, :]), r=["s_wk"], w=["tv"])
                V(lambda e: e.max_index(out=tix[:, hp, 8:16], in_max=tv[:, hp, 8:16], in_values=s_wk[:, hp, :]), r=["s_wk", "tv"], w=["tix"])
            V(lambda e: e.tensor_copy(out=tixf[:], in_=tix[:]), r=["tix"], w=["tixf"])
            tv4 = tv[:].rearrange("p (h two) k -> p h two k", two=2)
            cand4 = cand[:].rearrange("p h (a b) -> p h a b", a=16)
            V(lambda e: e.tensor_tensor(out=cand4, in0=tv4[:, :, 0, :].unsqueeze(3).to_broadcast([128, 8, 16, 16]),
                                        in1=tv4[:, :, 1, :].unsqueeze(2).to_broadcast([128, 8, 16, 16]), op=ALU.add),
              r=["tv"], w=["cand"])
            for hh in range(8):
                V(lambda e: e.max(out=cv[:, hh, 0:8], in_=cand[:, hh, :]), r=["cand"], w=["cv"])
                V(lambda e: e.max_index(out=cpos[:, hh, 0:8], in_max=cv[:, hh, 0:8], in_values=cand[:, hh, :]), r=["cand", "cv"], w=["cpos"])
                V(lambda e: e.match_replace(out=cwk[:, hh, :], in_to_replace=cv[:, hh, 0:8], in_values=cand[:, hh, :], imm_value=NEG),
                  r=["cand", "cv"], w=["s_wk"])
                V(lambda e: e.max(out=cv[:, hh, 8:16], in_=cwk[:, hh, :]), r=["s_wk"], w=["cv"])
                V(lambda e: e.max_index(out=cpos[:, hh, 8:16], in_max=cv[:, hh, 8:16], in_values=cwk[:, hh, :]), r=["s_wk", "cv"], w=["cpos"])
            V(lambda e: e.tensor_tensor(out=gw[:], in0=cv[:], in1=bc_mid(cv[:, :, 0], 16), op=ALU.subtract), r=["cv"], w=["gw"])
            A(lambda e: e.activation(out=gw[:], in_=gw[:], func=AF.Exp), r=["gw"], w=["gw"])
            V(lambda e: e.tensor_reduce(out=gst[:, 0:8], in_=gw[:], axis=AX.X, op=ALU.add), r=["gw"], w=["gst"])
            V(lambda e: e.reciprocal(out=gst[:, 8:16], in_=gst[:, 0:8]), r=["gst"], w=["gst"])
            V(lambda e: e.tensor_tensor(out=gw[:], in0=gw[:], in1=bc_mid(gst[:, 8:16], 16), op=ALU.mult), r=["gw", "gst"], w=["gw"])
            cpf = cpos[:].rearrange("p h k -> p (h k)")
            V(lambda e: e.tensor_single_scalar(out=cj[:, 0, :], in_=cpf, scalar=4, op=ALU.logical_shift_right), r=["cpos"], w=["cj"])
            V(lambda e: e.tensor_single_scalar(out=cj[:, 1, :], in_=cpf, scalar=15, op=ALU.bitwise_and), r=["cpos"], w=["cj"])
            V(lambda e: e.tensor_copy(out=cjf[:], in_=cj[:]), r=["cj"], w=["cjf"])
            tixf4 = tixf[:].rearrange("p (h two) k -> p h two k", two=2)
            for two in range(2):
                V(lambda e: e.tensor_tensor(out=oh[:], in0=bc_mid(cjf[:, two, :], 16),
                                            in1=iota16.unsqueeze(1).to_broadcast([128, 128, 16]), op=ALU.is_equal),
                  r=["cjf", "cst"], w=["oh"])
                oh4 = oh[:].rearrange("p (h k) j -> p h k j", h=8)
                oh24 = oh2[:].rearrange("p (h k) j -> p h k j", h=8)
                V(lambda e: e.tensor_tensor(out=oh24, in0=oh4,
                                            in1=tixf4[:, :, two, :].unsqueeze(2).to_broadcast([128, 8, 16, 16]), op=ALU.mult),
                  r=["oh", "tixf"], w=["oh2"])
                V(lambda e: e.tensor_reduce(out=iif[:, two, :], in_=oh2[:], axis=AX.X, op=ALU.add), r=["oh2"], w=["iif"])
            T(lambda e: e.transpose(out=ptf[:, 0:128], in_=iif[:, 0, :], identity=ident_f), r=["iif", "cst"], w=["ptf4"])
            T(lambda e: e.transpose(out=ptf[:, 128:256], in_=iif[:, 1, :], identity=ident_f), r=["iif", "cst"], w=["ptf4"])
            T(lambda e: e.transpose(out=ptf[:, 256:384], in_=gw[:].rearrange("p h k -> p (h k)"), identity=ident_f), r=["gw", "cst"], w=["ptf4"])
            sl = slice(it * 128, (it + 1) * 128)
            A(lambda e: e.copy(out=i1T_all[:, sl], in_=ptf[:, 0:128]), r=["ptf4"], w=["i1T_all"])
            A(lambda e: e.copy(out=i2T_all[:, sl], in_=ptf[:, 128:256]), r=["ptf4"], w=["i2T_all"])
            A(lambda e: e.copy(out=gT_all[:, sl], in_=ptf[:, 256:384]), r=["ptf4"], w=["gT_all"])

        rgen = {0: route_tile(0)}
        next(rgen[0])
        for it in range(NOWN):
            if it + 1 < NOWN:
                rgen[it + 1] = route_tile(it + 1)
                next(rgen[it + 1])
            for _ in rgen.pop(it):
                pass
    K.scope = scP
    S.barrier()

    S.set_reorder("3.2" in REORDER_PHASES)
    with phase_scope(K) as sc5:
        K.scope = sc5
        g2bc = K.sb("g2bc", [128, D], F32)
        DMA(lambda e: e.dma_start(out=g2bc[:], in_=modscr[0:1, 5 * D:6 * D].partition_broadcast(128)), r=["modscr"], w=["g2bc"])
        WTs = [K.sb("WT%d" % i, [128, TB, 128], BF16) for i in range(2)]
        SUBT = 8
        oh1 = Ring(K, "oh1_", [128, SUBT, 128], BF16, 3)
        oh2r = Ring(K, "oh2_", [128, SUBT, 128], BF16, 3)
        utr = Ring(K, "utr", [128, 2, 8, 128], BF16, 3)
        vr = Ring(K, "vr", [128, 2, D], BF16, 3)
        gt_r = Ring(K, "gt_r", [128, 2, TB], BF16, 2)
        pt_r = Ring(K, "pt_r", [128, 2, TB], BF16, 2)
        h2Tb = Ring(K, "h2Tb", [128, 8, TB], BF16, 2)
        x1t = Ring(K, "x1u", [128, 256], F32, 1)
        outt = Ring(K, "outu", [128, 256], F32, 1)
        pacc = K.ps("pacc", [128, 2, D], F32)
        pst = [K.ps("pst%d" % i, [128, 2, TB], F32) for i in range(2)]
        pw = [K.ps("pw%d" % i, [128, 4, 128], F32) for i in range(2)]
        iota_bc = iota128[:].unsqueeze(1).to_broadcast([128, SUBT, 128])
        NSUB = TB // SUBT
        pwc = [0]

        wb_state = {}

        def wbuild_gen(tb, sub):
            ts0 = tb * TB + sub * SUBT
            o1, o1n = oh1.next()
            o2, o2n = oh2r.next()
            V(lambda e: e.tensor_tensor(out=o1[:], in0=iota_bc, in1=bc_mid(i1T_all[:, ts0:ts0 + SUBT], 128), op=ALU.is_equal),
              r=["iota128", "i1T_all"], w=[o1n])
            V(lambda e: e.tensor_tensor(out=o2[:], in0=iota_bc, in1=bc_mid(i2T_all[:, ts0:ts0 + SUBT], 128), op=ALU.is_equal),
              r=["iota128", "i2T_all"], w=[o2n])
            G(lambda e: e.tensor_tensor(out=o2[:], in0=o2[:], in1=bc_mid(gT_all[:, ts0:ts0 + SUBT], 128), op=ALU.mult),
              r=[o2n, "gT_all"], w=[o2n])
            wb_state[(tb, sub)] = (o1, o1n, o2, o2n)

        def wbuild_mm(tb, sub):
            WT = WTs[tb % 2]
            wn = "WT%d" % (tb % 2)
            o1, o1n, o2, o2n = wb_state.pop((tb, sub))
            for q4 in range(SUBT // 4):
                pwi = pw[pwc[0] % 2]
                pwn = "pw%d" % (pwc[0] % 2)
                pwc[0] += 1
                for tt in range(4):
                    tl = q4 * 4 + tt
                    T(lambda e: e.matmul(pwi[:, tt, :], lhsT=o2[:, tl, :], rhs=o1[:, tl, :], start=True, stop=True),
                      r=[o1n, o2n], w=[pwn])
                tw0 = sub * SUBT + q4 * 4
                A(lambda e: e.copy(out=WT[:, tw0:tw0 + 4, :], in_=pwi[:]), r=[pwn], w=[wn])

        def wbuild_sub(tb, sub):
            wbuild_gen(tb, sub)
            wbuild_mm(tb, sub)

        def load_h2T(tb):
            hb_, hbn_ = h2Tb.next()
            for half in range(2):
                DMA(lambda e: e.dma_start(out=hb_[:, :, half * 128:(half + 1) * 128],
                                          in_=h2T_scr[tb * 2 + half].rearrange("p (k t) -> p k t", k=8)), r=["h2T_scr"], w=[hbn_])
            return hb_, hbn_

        for sub in range(NSUB):
            wbuild_sub(0, sub)
        hcur = load_h2T(0)
        for tb in range(NTB):
            t0 = tb * TB
            WT = WTs[tb % 2]
            wtn = "WT%d" % (tb % 2)
            h2T_blk, h2T_bn = hcur
            if tb + 1 < NTB:
                hcur = load_h2T(tb + 1)

            def sweep_load(pr_):
                ut_, utn = utr.next()
                DMA(lambda e: e.dma_start(out=ut_[:], in_=UT_scr[pr_].rearrange("p (c k e) -> p c k e", c=2, k=8)), r=["UT_scr"], w=[utn])
                vv, vvn = vr.next()
                DMA(lambda e: e.dma_start(out=vv[:], in_=V_scr[pr_].rearrange("p (c d) -> p c d", c=2)), r=["V_scr"], w=[vvn], q="act")
                return ut_, utn, vv, vvn

            def sweep_mm1(pr_, ut_, utn):
                ps_ = pst[pr_ % 2]
                psn = "pst%d" % (pr_ % 2)
                for ci in range(2):
                    for kc in range(8):
                        T(lambda e: e.matmul(ps_[:, ci, :], lhsT=ut_[:, ci, kc, :], rhs=h2T_blk[:, kc, :],
                                             start=(kc == 0), stop=(kc == 7)), r=[utn, h2T_bn], w=[psn])
                gt, gtn = gt_r.next()
                A(lambda e: e.activation(out=gt[:], in_=ps_[:], func=AF.Gelu), r=[psn], w=[gtn])
                pt_, ptn = pt_r.next()
                V(lambda e: e.tensor_tensor(out=pt_[:], in0=gt[:], in1=WT[:, :, pr_ * 2:pr_ * 2 + 2].rearrange("p t i -> p i t"), op=ALU.mult),
                  r=[gtn, wtn], w=[ptn])
                return pt_, ptn

            def sweep_mm2(pr_, pt_, ptn, vv, vvn):
                for ci in range(2):
                    for ts in range(2):
                        for dh in range(2):
                            T(lambda e: e.matmul(pacc[:, ts, dh * 512:(dh + 1) * 512], lhsT=pt_[:, ci, ts * 128:(ts + 1) * 128],
                                                 rhs=vv[:, ci, dh * 512:(dh + 1) * 512], start=(pr_ == 0 and ci == 0),
                                                 stop=(pr_ == 63 and ci == 1)), r=[ptn, vvn], w=["pacc"])

            lds = {0: sweep_load(0), 1: sweep_load(1)}
            pts = {0: sweep_mm1(0, lds[0][0], lds[0][1])}
            for pr_ in range(64):
                if pr_ + 2 < 64:
                    lds[pr_ + 2] = sweep_load(pr_ + 2)
                if pr_ + 1 < 64:
                    pts[pr_ + 1] = sweep_mm1(pr_ + 1, lds[pr_ + 1][0], lds[pr_ + 1][1])
                pt_, ptn = pts.pop(pr_)
                ut_, utn, vv, vvn = lds.pop(pr_)
                sweep_mm2(pr_, pt_, ptn, vv, vvn)
                if tb + 1 < NTB and pr_ % 2 == 1:
                    wbuild_gen(tb + 1, pr_ // 2)
                    if pr_ // 2 >= 1:
                        wbuild_mm(tb + 1, pr_ // 2 - 1)
            if tb + 1 < NTB:
                wbuild_mm(tb + 1, NSUB - 1)
            for ts in range(2):
                row0 = t0 + ts * 128
                for dh in range(4):
                    cs_ = slice(dh * 256, (dh + 1) * 256)
                    xt, xn = x1t.next()
                    DMA(lambda e: e.dma_start(out=xt[:], in_=x1scr[row0:row0 + 128, cs_]), r=["x1scr"], w=[xn])
                    ot, otn = outt.next()
                    V(lambda e: e.tensor_tensor(out=ot[:], in0=pacc[:, ts, cs_], in1=g2bc[:, cs_], op=ALU.mult), r=["pacc", "g2bc"], w=[otn])
                    V(lambda e: e.tensor_tensor(out=ot[:], in0=ot[:], in1=xt[:], op=ALU.add), r=[otn, xn], w=[otn])
                    DMA(lambda e: e.dma_start(out=out_d[row0:row0 + 128, cs_], in_=ot[:]), r=[otn], w=["out_d"])
    S.flush()
    scP.close()
    K.scope = K.root
    S.finish()
    K.root.close()
    return nc


def _host_inputs(inputs):
    f = lambda a: np.ascontiguousarray(np.asarray(a, dtype=np.float32))
    x, c, ctx, c_ctx = f(inputs["x"]), f(inputs["c"]), f(inputs["ctx"]), f(inputs["c_ctx"])
    T_ = x.shape[1]
    consts = np.zeros((128, 400), np.float32)
    r = np.arange(128)
    consts[:, 0:128] = np.eye(128, dtype=np.float32)
    consts[:, 128:256] = (r[:, None] <= r[None, :])
    consts[:, 256:384] = (r[:, None] >= r[None, :])
    consts[:, 384:400] = np.arange(16, dtype=np.float32)[None, :]
    inv = (10000.0 ** (-np.arange(16, dtype=np.float32) / 16)).astype(np.float32)
    shared = {
        "consts": consts,
        "nmgT": f(inputs["norm_mix_g"]).reshape(8, 128).T.copy(),
        "w_ada": f(inputs["w_ada"])[0], "b_ada": f(inputs["b_ada"]).reshape(1, -1),
        "norm_ffn_g": f(inputs["norm_ffn_g"]).reshape(1, -1), "w_in": f(inputs["w_in"])[0],
        "b_mgates": f(inputs["b_mgates"]).reshape(1, -1), "mlstm_norm_g": f(inputs["mlstm_norm_g"]).reshape(1, -1),
        "attn_q_norm_g": f(inputs["attn_q_norm_g"]).reshape(1, -1), "attn_k_norm_g": f(inputs["attn_k_norm_g"]).reshape(1, -1),
        "attn_sink": f(inputs["attn_sink"]).reshape(1, -1), "w_branch_m": f(inputs["w_branch_m"])[0],
        "w_branch_a": f(inputs["w_branch_a"])[0], "w_out": f(inputs["w_out"])[0],
        "peer_w_query": f(inputs["peer_w_query"])[0], "peer_sub_keys": f(inputs["peer_sub_keys"]).reshape(16 * 128, 128),
        "peer_u": f(inputs["peer_u"])[0], "peer_v": f(inputs["peer_v"])[0],
    }
    in_maps = []
    for core in range(8):
        b, j = core // 4, core % 4
        s0 = 2048 * j
        xo = np.zeros((NEXT * 128, D), np.float32)
        lo, hi = max(0, s0 - 128), min(T_, s0 + 2048 + 128)
        xo[lo - (s0 - 128):hi - (s0 - 128)] = x[b, lo:hi]
        xp = np.zeros((NPRE * 128, D), np.float32)
        pm = np.zeros((NPRE, 2), np.float32)
        xp[0:128], xp[128:256] = ctx[b, 0:128], ctx[b, 128:256]
        xp[256:384], xp[384:512] = ctx[b, 128:256], ctx[b, 0:128]
        pm[0], pm[1], pm[2], pm[3] = (1, 0), (1, 0), (0, 1), (0, 1)
        slot = 4
        for ch in range(0, 16 * j):
            xp[slot * 128:(slot + 1) * 128] = x[b, ch * 128:(ch + 1) * 128]
            pm[slot] = (1, 0)
            slot += 1
        for ch in range(63, 16 * (j + 1) - 1, -1):
            xp[slot * 128:(slot + 1) * 128] = x[b, ch * 128:(ch + 1) * 128]
            pm[slot] = (0, 1)
            slot += 1
        assert slot == NPRE
        cT = np.stack([c[b].reshape(8, 128).T, c_ctx.reshape(8, 128).T], axis=-1).reshape(128, 16)
        pos = (s0 - 128) + np.arange(NEXT * 128)
        row, col = pos // 64, pos % 64
        ang = np.concatenate([row[:, None].astype(np.float32) * inv, col[:, None].astype(np.float32) * inv], axis=-1)
        cs = np.stack([np.cos(ang), np.sin(ang)], axis=1).astype(np.float32)
        rope = cs.reshape(NEXT, 128, 64).transpose(1, 0, 2).reshape(128, NEXT * 64)
        am = np.zeros((128, 256), np.float32)
        am[:, 0:128] = consts[:, 256:384] * (1.0 if j > 0 else 0.0)
        am[:, 128:256] = consts[:, 128:256] * (1.0 if j < 3 else 0.0)
        m = dict(shared)
        m.update({"xown": xo, "xpre": xp, "pmask": np.broadcast_to(pm.reshape(1, -1), (128, NPRE * 2)).copy(),
                  "cT": np.ascontiguousarray(cT), "rope": np.ascontiguousarray(rope), "amask": am})
        in_maps.append(m)
    return in_maps


_NC_CACHE = {}


def kernel(**inputs):
    in_maps = _host_inputs(inputs)
    if 0 not in _NC_CACHE:
        _NC_CACHE[0] = build(0)
    res = run_bass_kernel_spmd(_NC_CACHE[0], in_maps, core_ids=list(range(8)))
    outs = [np.asarray(r["out"], dtype=np.float32) for r in res.results]
    out = np.zeros((2, 8192, D), np.float32)
    for core in range(8):
        b, j = core // 4, core % 4
        out[b, 2048 * j:2048 * (j + 1)] = outs[core]
    return out
```

```python
import contextlib
import numpy as np
import concourse.bass as bass
import concourse.mybir as mybir
from concourse.bass_utils import run_bass_kernel_spmd

F32 = mybir.dt.float32
BF16 = mybir.dt.bfloat16
U32 = mybir.dt.uint32
AF = mybir.ActivationFunctionType
ALU = mybir.AluOpType
AX = mybir.AxisListType

import os
ENGS = ("pe", "act", "dve", "pool", "sp")
REORDER_PHASES = set(os.environ.get("REORDER_PHASES", "").split(","))
REORDER_WINDOW = int(os.environ.get("REORDER_WINDOW", "100000"))
EPS = 1e-6
NPRE = 52
NOWN = 16
NEXT = 18
D = 1024
DIN = 4880
C_QM, C_KM, C_VM, C_OM, C_G, C_AQ, C_AK, C_AV, C_MG = 0, 512, 1024, 1536, 2048, 2064, 2576, 2704, 2832


class Sched:
    def __init__(self, nc, stack, n_dma_sems=48):
        self.nc = nc
        self.sems = {}
        names = ["s_" + e for e in ENGS if e != "sp"] + ["d%d" % i for i in range(n_dma_sems)]
        for n in names:
            self.sems[n] = stack.enter_context(nc.semaphore(n))
        self.engobj = {"pe": nc.tensor, "act": nc.scalar, "dve": nc.vector, "pool": nc.gpsimd, "sp": nc.sync}
        self.cnt = {e: 0 for e in ENGS}
        self.known = {e: {} for e in ENGS}
        self.pending = {e: {} for e in ENGS}
        self.bufs = {}
        self.n_dma_sems = n_dma_sems
        self.dma_next = 0
        self.dma_val = [0] * n_dma_sems
        self.dma_last_ev = [None] * n_dma_sems
        self.nops = 0
        self.reorder = False
        self.rec = []

    def set_reorder(self, flag):
        self.flush()
        self.reorder = flag

    def _b(self, name):
        b = self.bufs.get(name)
        if b is None:
            b = self.bufs[name] = {"w": None, "r": []}
        return b

    def latest_events(self):
        evs = []
        for e in ENGS:
            if e != "sp" and self.cnt[e] > 0:
                evs.append(("s_" + e, self.cnt[e]))
        for i in range(self.n_dma_sems):
            if self.dma_val[i] > 0:
                evs.append(("d%d" % i, self.dma_val[i]))
        return evs

    def barrier(self):
        self.flush()
        evs = self.latest_events()
        for e in ENGS:
            for (s, v) in evs:
                if self.pending[e].get(s, 0) < v:
                    self.pending[e][s] = v
        self.bufs = {}

    class _Probe:
        class _Ins:
            def then_inc(self, *a, **k):
                return self

        def __init__(self):
            self.calls = []

        def __getattr__(self, name):
            def f(*args, **kw):
                self.calls.append((name, args, kw))
                return Sched._Probe._Ins()
            return f

    @staticmethod
    def _free(ap):
        n = 1
        for d in ap.shape[1:]:
            n *= int(d)
        return n

    def _estimate(self, eng, call, dma):
        name, args, kw = call
        try:
            if dma:
                o = kw.get("out")
                nbytes = self._free(o) * int(o.shape[0]) * (4 if o.dtype == F32 or o.dtype == U32 else 2)
                return 0.12, 2.2 + nbytes / 1.2e5
            if name == "matmul":
                rhs = kw.get("rhs")
                n = self._free(rhs)
                t = 0.03 + max(n, 64) / 2000.0
                if rhs.dtype == F32:
                    t *= 4
                return t, t
            if name == "transpose":
                return 0.08, 0.08
            o = kw.get("out")
            if o is None:
                o = args[0]
            n = self._free(o)
            if eng == "dve":
                t = 0.2 + n / 960.0
            elif eng == "act":
                t = 0.22 + n / 1400.0 + (0.1 if kw.get("accum_out") is not None else 0.0)
            else:
                t = 0.3 + n / 500.0
            return t, t
        except Exception:
            return 0.5, 0.5

    def op(self, eng, fn, reads=(), writes=(), dma=False):
        if not self.reorder:
            return self._emit(eng, fn, reads, writes, dma)
        p = Sched._Probe()
        fn(p)
        assert len(p.calls) == 1
        call = p.calls[0]
        occ, lat = self._estimate(eng, call, dma)
        fn = (lambda e, c=call: getattr(e, c[0])(*c[1], **c[2]))
        reads, writes = tuple(reads), tuple(writes)
        if self.rec and eng == "pe" and not dma:
            last = self.rec[-1]
            if last[0] == "pe" and not last[4] and last[2] == reads and last[3] == writes:
                last[1].append(fn)
                last[5] += occ
                last[6] += lat
                return None
        self.rec.append([eng, [fn], reads, writes, dma, occ, lat])
        if len(self.rec) >= REORDER_WINDOW:
            self.flush()
        return None

    def flush(self):
        rec = self.rec
        self.rec = []
        n = len(rec)
        if n == 0:
            return
        import heapq
        succ = [[] for _ in range(n)]
        indeg = [0] * n
        lastw = {}
        readers = {}
        for i, (eng, fns, reads, writes, dma, occ, lat) in enumerate(rec):
            deps = set()
            for b in reads:
                if b in lastw:
                    deps.add(lastw[b])
            for b in writes:
                if b in lastw:
                    deps.add(lastw[b])
                for r_ in readers.get(b, ()):
                    deps.add(r_)
            deps.discard(i)
            for d in deps:
                succ[d].append(i)
            indeg[i] = len(deps)
            for b in reads:
                readers.setdefault(b, []).append(i)
            for b in writes:
                lastw[b] = i
                readers[b] = []
        ready = [0.0] * n
        free = {e: 0.0 for e in ENGS}
        fut = {e: [] for e in ENGS}
        now = {e: [] for e in ENGS}
        for i in range(n):
            if indeg[i] == 0:
                heapq.heappush(fut[rec[i][0]], (0.0, i))
        order = []
        while len(order) < n:
            best = None
            for e in ENGS:
                f, nw = fut[e], now[e]
                while f and f[0][0] <= free[e]:
                    heapq.heappush(nw, heapq.heappop(f)[1])
                if nw:
                    cand = (free[e], nw[0], e, True)
                elif f:
                    cand = (f[0][0], f[0][1], e, False)
                else:
                    continue
                if best is None or cand[:2] < best[:2]:
                    best = cand
            start, i, e, from_now = best
            if from_now:
                heapq.heappop(now[e])
            else:
                heapq.heappop(fut[e])
            order.append(i)
            free[e] = start + rec[i][5]
            done = start + rec[i][6]
            for j in succ[i]:
                rt = done + (0.1 if rec[j][0] == e else 0.2)
                if rt > ready[j]:
                    ready[j] = rt
                indeg[j] -= 1
                if indeg[j] == 0:
                    heapq.heappush(fut[rec[j][0]], (ready[j], j))
        if os.environ.get("REORDER_IDENTITY"):
            order = list(range(n))
        for i in order:
            eng, fns, reads, writes, dma, occ, lat = rec[i]
            self._emit(eng, fns, reads, writes, dma)

    def _emit(self, eng, fns, reads=(), writes=(), dma=False):
        if not isinstance(fns, (list, tuple)):
            fns = [fns]
        deps = set()
        for n in reads:
            b = self._b(n)
            if b["w"] is not None:
                deps.add(b["w"])
        for n in writes:
            b = self._b(n)
            if b["w"] is not None:
                deps.add(b["w"])
            for ev in b["r"]:
                deps.add(ev)
        for s, v in self.pending[eng].items():
            deps.add((s, v))
        self.pending[eng] = {}
        if dma:
            i = self.dma_next
            self.dma_next = (self.dma_next + 1) % self.n_dma_sems
            if self.dma_last_ev[i] is not None:
                deps.add(self.dma_last_ev[i])
            self.dma_val[i] += 16
            ev = ("d%d" % i, self.dma_val[i])
            self.dma_last_ev[i] = ev
            inc = 16
        else:
            self.cnt[eng] += 1
            ev = ("s_" + eng, self.cnt[eng])
            inc = 1
        waits = {}
        kn = self.known[eng]
        for (s, v) in deps:
            if eng == "pe" and s == "s_pe":
                continue
            if kn.get(s, 0) < v and waits.get(s, 0) < v:
                waits[s] = v
        e = self.engobj[eng]
        for (s, v) in sorted(waits.items()):
            kn[s] = v
            e.wait_ge(self.sems[s], v)
        for fn in fns[:-1]:
            fn(e)
        fns[-1](e).then_inc(self.sems[ev[0]], inc)
        self.nops += len(fns)
        for n in reads:
            self._b(n)["r"].append(ev)
        for n in writes:
            b = self._b(n)
            b["w"] = ev
            b["r"] = []
        return ev

    def finish(self):
        self.flush()
        e = self.engobj["sp"]
        for (s, v) in self.latest_events():
            e.wait_ge(self.sems[s], v)


class Ring:
    def __init__(self, K, name, shape, dt, n):
        self.t = [K.sb("%s%d" % (name, i), shape, dt) for i in range(n)]
        self.names = ["%s%d" % (name, i) for i in range(n)]
        self.n = n
        self.i = -1

    def next(self):
        self.i += 1
        return self.t[self.i % self.n], self.names[self.i % self.n]

    def cur(self):
        return self.t[self.i % self.n], self.names[self.i % self.n]


class Builder:
    def __init__(self, dbg=0):
        self.dbg = dbg
        self.nc = bass.Bass("TRN2", target_bir_lowering=False)
        self.root = contextlib.ExitStack()
        self.S = Sched(self.nc, self.root)
        self.scope = self.root

    def din(self, name, shape, dt=F32):
        return self.nc.dram_tensor(name, list(shape), dt, kind="ExternalInput").ap()

    def dout(self, name, shape, dt=F32):
        return self.nc.dram_tensor(name, list(shape), dt, kind="ExternalOutput").ap()

    def dscr(self, name, shape, dt=F32):
        return self.nc.dram_tensor(name, list(shape), dt).ap()

    def sb(self, name, shape, dt):
        return self.scope.enter_context(self.nc.sbuf_tensor(name, list(shape), dt))

    def ps(self, name, shape, dt):
        return self.scope.enter_context(self.nc.psum_tensor(name, list(shape), dt))

    def V(self, fn, r=(), w=()):
        return self.S.op("dve", fn, r, w)

    def A(self, fn, r=(), w=()):
        return self.S.op("act", fn, r, w)

    def G(self, fn, r=(), w=()):
        return self.S.op("pool", fn, r, w)

    def T(self, fn, r=(), w=()):
        return self.S.op("pe", fn, r, w)

    def DMA(self, fn, r=(), w=(), q="sp"):
        return self.S.op(q, fn, r, w, dma=True)


@contextlib.contextmanager
def phase_scope(K):
    with contextlib.ExitStack() as sc:
        yield sc
        K.S.flush()


def bc_mid(ap, n):
    return ap.unsqueeze(2).to_broadcast([ap.shape[0], ap.shape[1], n])


def build(dbg=0):
    K = Builder(dbg)
    nc, S = K.nc, K.S
    V, A, G, T, DMA = K.V, K.A, K.G, K.T, K.DMA

    xown = K.din("xown", [NEXT * 128, D])
    xpre = K.din("xpre", [NPRE * 128, D])
    pmask_d = K.din("pmask", [128, NPRE * 2])
    cT_d = K.din("cT", [128, 16])
    rope_d = K.din("rope", [128, NEXT * 64])
    amask_d = K.din("amask", [128, 256])
    consts_d = K.din("consts", [128, 3 * 128 + 16])
    nmgT_d = K.din("nmgT", [128, 8])
    w_ada = K.din("w_ada", [D, 6 * D])
    b_ada = K.din("b_ada", [1, 6 * D])
    nfg_d = K.din("norm_ffn_g", [1, D])
    w_in = K.din("w_in", [D, DIN])
    bmg_d = K.din("b_mgates", [1, 16])
    mng_d = K.din("mlstm_norm_g", [1, 512])
    qg_d = K.din("attn_q_norm_g", [1, 64])
    kg_d = K.din("attn_k_norm_g", [1, 64])
    sink_d = K.din("attn_sink", [1, 8])
    w_bm = K.din("w_branch_m", [512, D])
    w_ba = K.din("w_branch_a", [512, D])
    w_out = K.din("w_out", [D, D])
    w_q = K.din("peer_w_query", [D, 2048])
    skeys = K.din("peer_sub_keys", [16 * 128, 128])
    pu = K.din("peer_u", [16384, D])
    pv = K.din("peer_v", [16384, D])
    out_d = K.dout("out", [NOWN * 128, D])
    modscr = K.dscr("modscr", [2, 6 * D])
    UT_scr = K.dscr("UT_scr", [64, 128, 2 * 8 * 128], BF16)
    V_scr = K.dscr("V_scr", [64, 128, 2 * D], BF16)
    h2T_scr = K.dscr("h2T_scr", [NOWN, 128, 8 * 128], BF16)
    x1scr = K.dout("x1dbg", [NOWN * 128, D]) if dbg else K.dscr("x1scr", [NOWN * 128, D])
    if dbg:
        d_mod = K.dout("d_mod", [2, 6 * D])
        d_m = K.dout("d_m", [NOWN * 128, 512])
        d_a = K.dout("d_a", [NOWN * 128, 512])

    cst = K.sb("cst", [128, 3 * 128 + 16], F32)
    ident_f, trif_f, trir_f, iota16 = cst[:, 0:128], cst[:, 128:256], cst[:, 256:384], cst[:, 384:400]
    cstb = K.sb("cstb", [128, 3 * 128], BF16)
    ident_b, trif_b, trir_b = cstb[:, 0:128], cstb[:, 128:256], cstb[:, 256:384]
    ones_f = K.sb("ones_f", [128, 128], F32)
    amask_f = K.sb("amask_f", [128, 256], F32)
    amask_b = K.sb("amask_b", [128, 256], BF16)
    pmask = K.sb("pmask_sb", [128, NPRE * 2], F32)
    rope = K.sb("rope_sb", [128, NEXT * 64], F32)
    nmgT = K.sb("nmgT_sb", [128, 8], F32)
    featT = K.sb("featT", [128, 48 * 2], F32)
    gpT = K.sb("gpT", [128, 2 * 8], F32)
    shT = K.sb("shT", [128, 2 * 8], F32)
    bmg_bc = K.sb("bmg_bc", [128, 16], F32)
    mng_bc = K.sb("mng_bc", [128, 512], F32)
    qg_bc = K.sb("qg_bc", [128, 64], F32)
    kg_bc = K.sb("kg_bc", [128, 64], F32)
    esink = K.sb("esink", [128, 8], F32)
    epsb = K.sb("epsb", [128, 1], F32)

    DMA(lambda e: e.dma_start(out=cst[:], in_=consts_d[:, :]), w=["cst"])
    DMA(lambda e: e.dma_start(out=amask_f[:], in_=amask_d[:, :]), w=["amask_f"])
    DMA(lambda e: e.dma_start(out=pmask[:], in_=pmask_d[:, :]), w=["pmask"])
    DMA(lambda e: e.dma_start(out=rope[:], in_=rope_d[:, :]), w=["rope"])
    DMA(lambda e: e.dma_start(out=nmgT[:], in_=nmgT_d[:, :]), w=["nmgT"])
    DMA(lambda e: e.dma_start(out=bmg_bc[:], in_=bmg_d[0:1, :].partition_broadcast(128)), w=["bmg_bc"])
    DMA(lambda e: e.dma_start(out=mng_bc[:], in_=mng_d[0:1, :].partition_broadcast(128)), w=["mng_bc"])
    DMA(lambda e: e.dma_start(out=qg_bc[:], in_=qg_d[0:1, :].partition_broadcast(128)), w=["qg_bc"])
    DMA(lambda e: e.dma_start(out=kg_bc[:], in_=kg_d[0:1, :].partition_broadcast(128)), w=["kg_bc"])
    DMA(lambda e: e.dma_start(out=esink[:], in_=sink_d[0:1, :].partition_broadcast(128)), w=["esink"])
    V(lambda e: e.tensor_copy(out=cstb[:], in_=cst[:, 0:384]), r=["cst"], w=["cstb"])
    V(lambda e: e.tensor_copy(out=amask_b[:], in_=amask_f[:]), r=["amask_f"], w=["amask_b"])
    V(lambda e: e.memset(ones_f[:], 1.0), w=["ones_f"])
    V(lambda e: e.memset(epsb[:], EPS), w=["epsb"])
    A(lambda e: e.activation(out=esink[:], in_=esink[:], func=AF.Exp), r=["esink"], w=["esink"])

    def load_w_bf16(dst, dname, src, nk, c0, c1, stg_ring, dst_c0=None):
        if dst_c0 is None:
            dst_c0 = c0
        srcv = src.rearrange("(kc p) n -> p kc n", p=128)
        blocks = list(range(c0, c1, 512))
        names = []
        for bi, cc in enumerate(blocks):
            ce = min(cc + 512, c1)
            dv = dst[:, :, dst_c0 + (cc - c0):dst_c0 + (ce - c0)]
            last = bi == len(blocks) - 1
            nm = "%s_blk%d_%d" % (dname, c0, bi)
            if last:
                DMA(lambda e: e.dma_start(out=dv, in_=srcv[:, :, cc:ce]), r=names, w=[dname, nm], q="pool")
            else:
                DMA(lambda e: e.dma_start(out=dv, in_=srcv[:, :, cc:ce]), w=[nm], q="pool")
                names.append(nm)

    S.set_reorder("0" in REORDER_PHASES)
    with phase_scope(K) as sc0:
        K.scope = sc0
        cT = K.sb("cT_sb", [128, 16], F32)
        scT = K.sb("scT", [128, 16], F32)
        mod_sb = K.sb("mod_sb", [2, 6 * D], F32)
        bada2 = K.sb("bada2", [2, 6 * D], F32)
        sel = K.sb("sel", [2, 128], F32)
        wa = Ring(K, "wa", [128, 8 * 512], F32, 2)
        pmod = K.ps("pmod", [128, 512], F32)
        pft = K.ps("pft", [128, 96], F32)
        DMA(lambda e: e.dma_start(out=cT[:], in_=cT_d[:, :]), w=["cT"])
        DMA(lambda e: e.dma_start(out=bada2[:], in_=b_ada[0:1, :].partition_broadcast(2)), w=["bada2"])
        A(lambda e: e.activation(out=scT[:], in_=cT[:], func=AF.Silu), r=["cT"], w=["scT"])
        scT3 = scT[:].rearrange("p (k c) -> p k c", c=2)
        wav = w_ada.rearrange("(kc p) n -> p kc n", p=128)
        for nb in range(12):
            wt, wn = wa.next()
            wv = wt[:].rearrange("p (k n) -> p k n", k=8)
            DMA(lambda e: e.dma_start(out=wv, in_=wav[:, :, nb * 512:(nb + 1) * 512]), w=[wn])
            for kc in range(8):
                T(lambda e: e.matmul(pmod[0:2, :], lhsT=scT3[:, kc, :], rhs=wv[:, kc, :], start=(kc == 0), stop=(kc == 7)),
                  r=["scT", wn], w=["pmod"])
            V(lambda e: e.tensor_tensor(out=mod_sb[:, nb * 512:(nb + 1) * 512], in0=pmod[0:2, :],
                                        in1=bada2[:, nb * 512:(nb + 1) * 512], op=ALU.add),
              r=["pmod", "bada2"], w=["mod_sb"])
        DMA(lambda e: e.dma_start(out=modscr[:, :], in_=mod_sb[:]), r=["mod_sb"], w=["modscr"])
        if dbg:
            DMA(lambda e: e.dma_start(out=d_mod[:, :], in_=mod_sb[:]), r=["mod_sb"])
        for jc in range(48):
            T(lambda e: e.transpose(out=pft[:, jc * 2:jc * 2 + 2], in_=mod_sb[0:2, jc * 128:(jc + 1) * 128],
                                    identity=ident_f[0:2, 0:2]),
              r=["mod_sb", "cst"], w=["pft"])
        V(lambda e: e.tensor_copy(out=featT[:], in_=pft[:]), r=["pft"], w=["featT"])
        f3 = featT[:].rearrange("p (j c o) -> p j c o", j=6, c=8)
        for cond in range(2):
            V(lambda e: e.scalar_tensor_tensor(out=gpT[:, cond * 8:(cond + 1) * 8], in0=f3[:, 1, :, cond], scalar=1.0,
                                               in1=nmgT[:], op0=ALU.add, op1=ALU.mult),
              r=["featT", "nmgT"], w=["gpT"])
            V(lambda e: e.tensor_copy(out=shT[:, cond * 8:(cond + 1) * 8], in_=f3[:, 0, :, cond]), r=["featT"], w=["shT"])
    K.scope = K.root
    S.barrier()

    def make_hT(K, src_rows, cond, R, ptn, pt):
        xt, xn = R["xt"].next()
        DMA(lambda e: e.dma_start(out=xt[:], in_=src_rows), w=[xn])
        ss, ssn = R["ss"].next()
        A(lambda e: e.activation(out=R["junk"][:], in_=xt[:], func=AF.Square, accum_out=ss[:, 0:1]),
          r=[xn], w=["junk", ssn])
        A(lambda e: e.activation(out=ss[:, 1:2], in_=ss[:, 0:1], func=AF.Ln, scale=1.0 / D, bias=epsb[:, 0:1]),
          r=[ssn, "epsb"], w=[ssn])
        A(lambda e: e.activation(out=ss[:, 2:3], in_=ss[:, 1:2], func=AF.Exp, scale=-0.5), r=[ssn], w=[ssn])
        xs, xsn = R["xs"].next()
        A(lambda e: e.activation(out=xs[:], in_=xt[:], func=AF.Identity, scale=ss[:, 2:3]), r=[xn, ssn], w=[xsn])
        for kc in range(8):
            T(lambda e: e.transpose(out=pt[:, kc, :], in_=xs[:, kc * 128:(kc + 1) * 128], identity=ident_b),
              r=[xsn, "cstb"], w=[ptn])
        hT, hn = R["hT"].next()
        V(lambda e: e.tensor_tensor(out=hT[:], in0=pt[:], in1=bc_mid(gpT[:, cond * 8:(cond + 1) * 8], 128), op=ALU.mult),
          r=[ptn, "gpT"], w=[hn])
        V(lambda e: e.tensor_tensor(out=hT[:], in0=hT[:], in1=bc_mid(shT[:, cond * 8:(cond + 1) * 8], 128), op=ALU.add),
          r=[hn, "shT"], w=[hn])
        return hT, hn, xt, xn

    def gate_pre(K, gps, gpsn, R):
        g, gn = R["g"].next()
        V(lambda e: e.tensor_tensor(out=g[:, 0:16], in0=gps[:, 0:16], in1=bmg_bc[:], op=ALU.add), r=[gpsn, "bmg_bc"], w=[gn])
        return {"g": g, "gn": gn}

    def gate_mid(K, G_, cps, cpsn, slot_mask=None):
        g, gn = G_["g"], G_["gn"]
        gp4 = g[:, 0:16].rearrange("p (d k h) -> p d k h", d=2, k=2)
        ef = g[:, 16:24].rearrange("p (d h) -> p d h", d=2)
        A(lambda e: e.activation(out=ef, in_=gp4[:, :, 1, :], func=AF.Exp, scale=-1.0), r=[gn], w=[gn])
        sp = g[:, 24:32]
        A(lambda e: e.activation(out=sp, in_=g[:, 16:24], func=AF.Ln, bias=1.0), r=[gn], w=[gn])
        if slot_mask is not None:
            sp3 = sp.rearrange("p (d h) -> p d h", d=2)
            V(lambda e: e.tensor_tensor(out=sp3, in0=sp3, in1=bc_mid(slot_mask, 4), op=ALU.mult), r=[gn, "pmask"], w=[gn])
        T(lambda e: e.matmul(cps[:, 0:4], lhsT=trif_f, rhs=sp[:, 0:4], start=True, stop=True), r=[gn, "cst"], w=[cpsn])
        T(lambda e: e.matmul(cps[:, 4:8], lhsT=trir_f, rhs=sp[:, 4:8], start=True, stop=True), r=[gn, "cst"], w=[cpsn])
        T(lambda e: e.matmul(cps[:, 8:16], lhsT=ones_f[:], rhs=sp, start=True, stop=True), r=[gn, "ones_f"], w=[cpsn])
        cs = g[:, 32:48]
        V(lambda e: e.tensor_copy(out=cs, in_=cps[:, 0:16]), r=[cpsn], w=[gn])

    def gate_post(K, G_, slot_mask=None):
        g, gn = G_["g"], G_["gn"]
        gp4 = g[:, 0:16].rearrange("p (d k h) -> p d k h", d=2, k=2)
        Bc, At = g[:, 32:40], g[:, 40:48]
        t1 = g[:, 48:56]
        V(lambda e: e.scalar_tensor_tensor(out=t1, in0=At, scalar=-0.5, in1=Bc, op0=ALU.mult, op1=ALU.add), r=[gn], w=[gn])
        argb = g[:, 56:64]
        V(lambda e: e.tensor_tensor(out=argb.rearrange("p (d h) -> p d h", d=2), in0=t1.rearrange("p (d h) -> p d h", d=2),
                                    in1=gp4[:, :, 0, :], op=ALU.add), r=[gn], w=[gn])
        beta = g[:, 64:72]
        A(lambda e: e.activation(out=beta, in_=argb, func=AF.Exp), r=[gn], w=[gn])
        if slot_mask is not None:
            b3 = beta.rearrange("p (d h) -> p d h", d=2)
            V(lambda e: e.scalar_tensor_tensor(out=b3, in0=b3, scalar=128.0 ** -0.5, in1=bc_mid(slot_mask, 4),
                                               op0=ALU.mult, op1=ALU.mult), r=[gn, "pmask"], w=[gn])
        else:
            V(lambda e: e.tensor_scalar(out=beta, in0=beta, scalar1=128.0 ** -0.5, scalar2=None, op0=ALU.mult), r=[gn], w=[gn])
        ea, eh, alpha = g[:, 72:80], g[:, 80:88], g[:, 88:96]
        A(lambda e: e.activation(out=ea, in_=At, func=AF.Exp, scale=-1.0), r=[gn], w=[gn])
        A(lambda e: e.activation(out=eh, in_=At, func=AF.Exp, scale=-0.5), r=[gn], w=[gn])
        A(lambda e: e.activation(out=alpha, in_=t1, func=AF.Exp, scale=-1.0), r=[gn], w=[gn])
        return gn, beta, ea, eh, alpha

    def gate_math(K, gps, gpsn, R, slot_mask=None):
        G_ = gate_pre(K, gps, gpsn, R)
        gate_mid(K, G_, gps[:, 16:32], gpsn, slot_mask)
        return gate_post(K, G_, slot_mask)

    def make_vt(K, vps, vpsn, beta, gn, R):
        res = []
        for d in range(2):
            vt, vn = R["vt%d" % d].next()
            bd = beta[:, d * 4:(d + 1) * 4]
            V(lambda e: e.tensor_tensor(out=vt[:, :, 0:128], in0=vps.rearrange("p (h v) -> p h v", h=4),
                                        in1=bc_mid(bd, 128), op=ALU.mult), r=[vpsn, gn], w=[vn])
            V(lambda e: e.tensor_copy(out=vt[:, :, 128:129], in_=bd.unsqueeze(2)), r=[gn], w=[vn])
            res.append((vt, vn))
        return res

    S.set_reorder("1" in REORDER_PHASES)
    scB = contextlib.ExitStack()
    K.scope = scB
    mT_all = K.sb("mT_all", [128, 4, NOWN * 128], BF16)
    aT_all = K.sb("aT_all", [128, 4, NOWN * 128], BF16)
    scA = contextlib.ExitStack()
    K.scope = scA
    snapF = K.sb("snapF", [128, NOWN * 4 * 129], BF16)
    snapR = K.sb("snapR", [128, NOWN * 4 * 129], BF16)
    snapF4 = snapF[:].rearrange("p (c h n) -> p c h n", c=NOWN, h=4)
    snapR4 = snapR[:].rearrange("p (c h n) -> p c h n", c=NOWN, h=4)
    with phase_scope(K) as sc1:
        K.scope = sc1
        wkv = K.sb("wkv", [128, 8, 1040], BF16)
        stg = Ring(K, "stg", [128, 1024], F32, 2)
        load_w_bf16(wkv, "wkv", w_in, 8, C_KM, C_KM + 1024, stg, dst_c0=0)
        load_w_bf16(wkv, "wkv", w_in, 8, C_G, C_G + 16, stg, dst_c0=1024)
        R = {"xt": Ring(K, "xt", [128, D], F32, 3), "ss": Ring(K, "ss", [128, 4], F32, 3),
             "xs": Ring(K, "xs", [128, D], BF16, 2), "hT": Ring(K, "hT", [128, 8, 128], BF16, 2),
             "g": Ring(K, "g", [128, 96], F32, 4), "vt0": Ring(K, "vt0_", [128, 4, 129], BF16, 2),
             "vt1": Ring(K, "vt1_", [128, 4, 129], BF16, 2), "junk": K.sb("junk", [128, D], BF16)}
        ksb = Ring(K, "ksb", [128, 512], BF16, 4)
        tmpu = Ring(K, "tmpu", [128, 4, 129], F32, 2)
        Cst = [K.sb("Cst%d" % d, [128, 4, 129], F32) for d in range(2)]
        contribR = K.sb("contribR", [128, NOWN * 4 * 129], F32)
        contribR4 = contribR[:].rearrange("p (c h n) -> p c h n", c=NOWN, h=4)
        eaR = K.sb("eaR", [128, NOWN * 4], F32)
        ehR = K.sb("ehR", [128, NOWN * 4], F32)
        pt = K.ps("pt1", [128, 8, 128], BF16)
        pk = K.ps("pk1", [128, 512], F32)
        pv_ = K.ps("pv1", [128, 512], F32)
        pg = K.ps("pg1", [128, 512], F32)
        pc = K.ps("pc1", [128, 8, 256], F32)
        for d in range(2):
            V(lambda e: e.memset(Cst[d][:], 0.0), w=["Cst%d" % d])
        pm3 = pmask[:].rearrange("p (s d) -> p s d", d=2)
        ub = Ring(K, "ub", [128, 2, D], BF16, 2)
        vb = Ring(K, "vb", [128, 2, D], BF16, 2)
        utb = Ring(K, "utb", [128, 2, 8, 128], BF16, 2)
        puv = pu.rearrange("(g c p) d -> g p c d", c=2, p=128)
        pvv = pv.rearrange("(g c p) d -> g p c d", c=2, p=128)
        conv_state = {}

        def conv_in(gI):
            vt_, vtn = vb.next()
            DMA(lambda e: e.dma_start(out=vt_[:], in_=pvv[gI]), w=[vtn], q="pool")
            ut_, utn = ub.next()
            DMA(lambda e: e.dma_start(out=ut_[:], in_=puv[gI]), w=[utn], q="pool")
            uo, uon = utb.next()
            for ci in range(2):
                for kc in range(8):
                    T(lambda e: e.transpose(out=pt[:, kc, :], in_=ut_[:, ci, kc * 128:(kc + 1) * 128], identity=ident_b),
                      r=[utn, "cstb"], w=["pt1"])
                A(lambda e: e.copy(out=uo[:, ci], in_=pt[:]), r=["pt1"], w=[uon])
            conv_state[gI] = (vt_, vtn, uo, uon)

        def conv_out(gI):
            vt_, vtn, uo, uon = conv_state.pop(gI)
            DMA(lambda e: e.dma_start(out=V_scr[gI].rearrange("p (c d) -> p c d", c=2), in_=vt_[:]), r=[vtn], w=["V_scr"], q="pool")
            DMA(lambda e: e.dma_start(out=UT_scr[gI].rearrange("p (c k e) -> p c k e", c=2, k=8), in_=uo[:]), r=[uon], w=["UT_scr"], q="pool")
        vsb = Ring(K, "vsb", [128, 512], BF16, 4)
        tiles = {}

        def slot_info(slot):
            own = slot >= NPRE
            c = slot - NPRE
            if own:
                return own, c, xown[(c + 1) * 128:(c + 2) * 128, :], 0, None
            return own, c, xpre[slot * 128:(slot + 1) * 128, :], (1 if slot < 4 else 0), pm3[:, slot, :]

        def st1(slot):
            own, c, rows, cond, smask = slot_info(slot)
            hT, hn, xt, xn = make_hT(K, rows, cond, R, "pt1", pt)
            if slot < 64:
                conv_in(slot)
            if 1 <= slot <= 64:
                conv_out(slot - 1)
            for kc in range(8):
                T(lambda e: e.matmul(pk[:], lhsT=hT[:, kc, :], rhs=wkv[:, kc, 0:512], start=(kc == 0), stop=(kc == 7)),
                  r=[hn, "wkv"], w=["pk1"])
            for kc in range(8):
                T(lambda e: e.matmul(pv_[:], lhsT=hT[:, kc, :], rhs=wkv[:, kc, 512:1024], start=(kc == 0), stop=(kc == 7)),
                  r=[hn, "wkv"], w=["pv1"])
            for kc in range(8):
                T(lambda e: e.matmul(pg[:, 0:16], lhsT=hT[:, kc, :], rhs=wkv[:, kc, 1024:1040], start=(kc == 0), stop=(kc == 7)),
                  r=[hn, "wkv"], w=["pg1a"])
            kt, kn = ksb.next()
            A(lambda e: e.copy(out=kt[:], in_=pk[:]), r=["pk1"], w=[kn])
            vt_, vtn = vsb.next()
            A(lambda e: e.copy(out=vt_[:], in_=pv_[:]), r=["pv1"], w=[vtn])
            G_ = gate_pre(K, pg, "pg1a", R)
            tiles[slot] = {"kt": kt, "kn": kn, "v": vt_, "vn": vtn, "G": G_}

        def st2(slot):
            own, c, rows, cond, smask = slot_info(slot)
            gate_mid(K, tiles[slot]["G"], pg[:, 16:32], "pg1b", smask)

        def st3(slot):
            own, c, rows, cond, smask = slot_info(slot)
            tl = tiles.pop(slot)
            kt, kn = tl["kt"], tl["kn"]
            gn, beta, ea, eh, alpha = gate_post(K, tl["G"], smask)
            vts = make_vt(K, tl["v"][:], tl["vn"], beta, gn, R)
            for d in range(2):
                vt, vn = vts[d]
                for h in range(4):
                    T(lambda e: e.matmul(pc[:, d * 4 + h, 0:129], lhsT=kt[:, h * 128:(h + 1) * 128], rhs=vt[:, h, :],
                                         start=True, stop=True), r=[kn, vn], w=["pc1_%d" % d])
            for d in range(2):
                ead, ehd = ea[:, d * 4:(d + 1) * 4], eh[:, d * 4:(d + 1) * 4]
                cn = "Cst%d" % d
                if own and d == 0:
                    V(lambda e: e.tensor_tensor(out=snapF4[:, c], in0=Cst[0][:], in1=bc_mid(ehd, 129), op=ALU.mult),
                      r=[cn, gn], w=["snapF"])
                if own and d == 1:
                    V(lambda e: e.tensor_tensor(out=contribR4[:, c], in0=pc[:, 4:8, 0:129], in1=bc_mid(ehd, 129), op=ALU.mult),
                      r=["pc1_1", gn], w=["contribR"])
                    V(lambda e: e.tensor_copy(out=eaR[:, c * 4:(c + 1) * 4], in_=ead), r=[gn], w=["eaR"])
                    V(lambda e: e.tensor_copy(out=ehR[:, c * 4:(c + 1) * 4], in_=ehd), r=[gn], w=["ehR"])
                    continue
                tu, tn = tmpu.next()
                V(lambda e: e.tensor_tensor(out=tu[:], in0=pc[:, d * 4:(d + 1) * 4, 0:129], in1=bc_mid(ehd, 129), op=ALU.mult),
                  r=["pc1_%d" % d, gn], w=[tn])
                V(lambda e: e.tensor_tensor(out=Cst[d][:], in0=Cst[d][:], in1=bc_mid(ead, 129), op=ALU.mult), r=[cn, gn], w=[cn])
                V(lambda e: e.tensor_tensor(out=Cst[d][:], in0=Cst[d][:], in1=tu[:], op=ALU.add), r=[cn, tn], w=[cn])

        NS = NPRE + NOWN
        for it in range(NS + 2):
            if it < NS:
                st1(it)
            if 0 <= it - 2 < NS:
                st3(it - 2)
            if 0 <= it - 1 < NS:
                st2(it - 1)
        for c in range(NOWN - 1, -1, -1):
            ead, ehd = eaR[:, c * 4:(c + 1) * 4], ehR[:, c * 4:(c + 1) * 4]
            V(lambda e: e.tensor_tensor(out=snapR4[:, c], in0=Cst[1][:], in1=bc_mid(ehd, 129), op=ALU.mult),
              r=["Cst1", "ehR"], w=["snapR"])
            V(lambda e: e.tensor_tensor(out=Cst[1][:], in0=Cst[1][:], in1=bc_mid(ead, 129), op=ALU.mult), r=["Cst1", "eaR"], w=["Cst1"])
            V(lambda e: e.tensor_tensor(out=Cst[1][:], in0=Cst[1][:], in1=contribR4[:, c], op=ALU.add), r=["Cst1", "contribR"], w=["Cst1"])
    K.scope = K.root
    S.barrier()

    S.set_reorder("2a" in REORDER_PHASES)
    with phase_scope(K) as sc2:
        K.scope = sc2
        NA = C_MG
        wA = K.sb("wA", [128, 8, NA], BF16)
        stg = Ring(K, "stg2", [128, 1024], F32, 2)
        load_w_bf16(wA, "wA", w_in, 8, 0, NA, stg)
        R = {"xt": Ring(K, "xt2", [128, D], F32, 2), "ss": Ring(K, "ss2", [128, 4], F32, 3),
             "xs": Ring(K, "xs2", [128, D], BF16, 2), "hT": Ring(K, "hT2", [128, 8, 128], BF16, 2),
             "g": Ring(K, "g2", [128, 96], F32, 2), "vt0": Ring(K, "vt02_", [128, 4, 129], BF16, 2),
             "vt1": Ring(K, "vt12_", [128, 4, 129], BF16, 2), "junk": K.sb("junk2", [128, D], BF16)}
        kT_all = K.sb("kT_all", [128, 20, 128], BF16)
        v_all = K.sb("v_all", [128, 20, 2, 65], BF16)
        qTa = Ring(K, "qTa", [128, 4, 128], BF16, 3)
        qTm = Ring(K, "qTm", [128, 4, 128], BF16, 2)
        kTm = Ring(K, "kTm", [128, 4, 128], BF16, 2)
        og = Ring(K, "og", [128, 512], F32, 2)
        smf = Ring(K, "smf", [128, 4, 128], BF16, 2)
        smr = Ring(K, "smr", [128, 4, 128], BF16, 2)
        pr = Ring(K, "pr", [128, 4, 128], BF16, 10)
        wk = Ring(K, "wk", [128, 512], F32, 6)
        sm = Ring(K, "sm", [128, 32], F32, 6)
        mb = Ring(K, "mb", [128, 512], BF16, 3)
        pt = K.ps("pt2", [128, 8, 128], BF16)
        pab = K.ps("pab2", [128, 1024], F32)
        pn = K.ps("pn2", [128, 8, 256], F32)
        pcs = K.ps("pcs2", [128, 512], F32)
        pabi = [0]

        def proj(hT, hn, c0, width, transposed=False):
            i = pabi[0] % 2
            pabi[0] += 1
            nm = "pab2_%d" % i
            if not transposed:
                outp = pab[:, i * 512:i * 512 + width]
                for kc in range(8):
                    T(lambda e: e.matmul(outp, lhsT=hT[:, kc, :], rhs=wA[:, kc, c0:c0 + width], start=(kc == 0), stop=(kc == 7)),
                      r=[hn, "wA"], w=[nm])
            else:
                outp = pab[:, i * 512:(i + 1) * 512]
                for hh in range(width // 128):
                    for kc in range(8):
                        T(lambda e: e.matmul(outp[:, hh * 128:(hh + 1) * 128], lhsT=wA[:, kc, c0 + hh * 128:c0 + (hh + 1) * 128],
                                             rhs=hT[:, kc, :], start=(kc == 0), stop=(kc == 7)), r=[hn, "wA"], w=[nm])
            return outp, nm

        V(lambda e: e.memset(v_all[:], 1.0), w=["v_all"])

        def qk_norm_rope(src, srcn, G_, J_, gbc, gbn, e_idx, out_ap, outn):
            nh = G_ * J_
            sq, sqn = wk.next()
            A(lambda e: e.activation(out=sq[:, 0:nh * 64], in_=src, func=AF.Square), r=[srcn], w=[sqn])
            st_, stn = sm.next()
            V(lambda e: e.tensor_reduce(out=st_[:, 0:nh], in_=sq[:, 0:nh * 64].rearrange("p (h d) -> p h d", h=nh),
                                        axis=AX.X, op=ALU.add), r=[sqn], w=[stn])
            A(lambda e: e.activation(out=st_[:, 8:8 + nh], in_=st_[:, 0:nh], func=AF.Ln, scale=1.0 / 64, bias=epsb[:, 0:1]),
              r=[stn, "epsb"], w=[stn])
            A(lambda e: e.activation(out=st_[:, 16:16 + nh], in_=st_[:, 8:8 + nh], func=AF.Exp, scale=-0.5), r=[stn], w=[stn])
            qn, qnn = wk.next()
            qn3 = qn[:, 0:nh * 64].rearrange("p (h d) -> p h d", h=nh)
            V(lambda e: e.tensor_tensor(out=qn3, in0=src.rearrange("p (h d) -> p h d", h=nh), in1=bc_mid(st_[:, 16:16 + nh], 64),
                                        op=ALU.mult), r=[srcn, stn], w=[qnn])
            V(lambda e: e.tensor_tensor(out=qn3, in0=qn3, in1=gbc[:].unsqueeze(1).to_broadcast([128, nh, 64]), op=ALU.mult),
              r=[qnn, gbn], w=[qnn])
            qn4 = qn[:, 0:nh * 64].rearrange("p (g j d) -> p g j d", g=G_, j=J_)
            o4 = out_ap.rearrange("p (j g d) -> p g j d", j=J_, g=G_)
            if e_idx is None:
                V(lambda e: e.tensor_copy(out=o4, in_=qn4), r=[qnn], w=[outn])
                return
            cos = rope[:, e_idx * 64:e_idx * 64 + 32].unsqueeze(1).to_broadcast([128, nh, 32])
            sin = rope[:, e_idx * 64 + 32:e_idx * 64 + 64].unsqueeze(1).to_broadcast([128, nh, 32])
            tw, twn = wk.next()
            tA = tw[:, 0:nh * 32].rearrange("p (h d) -> p h d", h=nh)
            tB = tw[:, 256:256 + nh * 32].rearrange("p (h d) -> p h d", h=nh)
            q1, q2 = qn3[:, :, 0:32], qn3[:, :, 32:64]
            V(lambda e: e.tensor_tensor(out=tA, in0=q1, in1=cos, op=ALU.mult), r=[qnn, "rope"], w=[twn])
            V(lambda e: e.tensor_tensor(out=tB, in0=q2, in1=sin, op=ALU.mult), r=[qnn, "rope"], w=[twn])
            tA4 = tw[:, 0:nh * 32].rearrange("p (g j d) -> p g j d", g=G_, j=J_)
            tB4 = tw[:, 256:256 + nh * 32].rearrange("p (g j d) -> p g j d", g=G_, j=J_)
            V(lambda e: e.tensor_tensor(out=o4[:, :, :, 0:32], in0=tA4, in1=tB4, op=ALU.subtract), r=[twn], w=[outn])
            tw2, twn2 = wk.next()
            tC = tw2[:, 0:nh * 32].rearrange("p (h d) -> p h d", h=nh)
            tD = tw2[:, 256:256 + nh * 32].rearrange("p (h d) -> p h d", h=nh)
            V(lambda e: e.tensor_tensor(out=tC, in0=q2, in1=cos, op=ALU.mult), r=[qnn, "rope"], w=[twn2])
            V(lambda e: e.tensor_tensor(out=tD, in0=q1, in1=sin, op=ALU.mult), r=[qnn, "rope"], w=[twn2])
            tC4 = tw2[:, 0:nh * 32].rearrange("p (g j d) -> p g j d", g=G_, j=J_)
            tD4 = tw2[:, 256:256 + nh * 32].rearrange("p (g j d) -> p g j d", g=G_, j=J_)
            V(lambda e: e.tensor_tensor(out=o4[:, :, :, 32:64], in0=tC4, in1=tD4, op=ALU.add), r=[twn2], w=[outn])

        def attn_kv(hT, hn, e_store, e_rope):
            ps_, pn_ = proj(hT, hn, C_AK, 256)
            kr, krn = mb.next()
            qk_norm_rope(ps_[:, 0:128], pn_, 2, 1, kg_bc, "kg_bc", e_rope, kr[:, 0:128], krn)
            V(lambda e: e.tensor_copy(out=v_all[:, e_store, :, 0:64], in_=ps_[:, 128:256].rearrange("p (h d) -> p h d", h=2)),
              r=[pn_], w=["v_all"])
            T(lambda e: e.transpose(out=pt[:, 0, :], in_=kr[:, 0:128], identity=ident_b), r=[krn, "cstb"], w=["pt2"])
            V(lambda e: e.tensor_copy(out=kT_all[:, e_store, :], in_=pt[:, 0, :]), r=["pt2"], w=["kT_all"])

        def own_part(c, hT, hn, e_idx):
            gps, gpn = proj(hT, hn, C_G, 16)
            gfull = pab[:, (pabi[0] - 1) % 2 * 512:((pabi[0] - 1) % 2 + 1) * 512]
            gn, beta, ea, eh, alpha = gate_math(K, gfull, gpn, R, None)
            vps, vpn = proj(hT, hn, C_VM, 512)
            vts = make_vt(K, vps, vpn, beta, gn, R)
            qps, qpn = proj(hT, hn, C_QM, 512, transposed=True)
            qt, qtn = qTm.next()
            A(lambda e: e.copy(out=qt[:], in_=qps.rearrange("p (h t) -> p h t", h=4)), r=[qpn], w=[qtn])
            kps, kpn = proj(hT, hn, C_KM, 512, transposed=True)
            ktm, ktn = kTm.next()
            A(lambda e: e.copy(out=ktm[:], in_=kps.rearrange("p (h t) -> p h t", h=4)), r=[kpn], w=[ktn])
            ops, opn = proj(hT, hn, C_OM, 512)
            ogt, ogn = og.next()
            A(lambda e: e.activation(out=ogt[:], in_=ops, func=AF.Sigmoid), r=[opn], w=[ogn])
            V(lambda e: e.tensor_tensor(out=ogt[:], in0=ogt[:], in1=mng_bc[:], op=ALU.mult), r=[ogn, "mng_bc"], w=[ogn])
            for h in range(4):
                T(lambda e: e.matmul(pcs[:, h * 128:(h + 1) * 128], lhsT=ktm[:, h, :], rhs=qt[:, h, :], start=True, stop=True),
                  r=[ktn, qtn], w=["pcs2"])
            sf, sfn = smf.next()
            sr, srn = smr.next()
            pcs3 = pcs[:].rearrange("p (h t) -> p h t", h=4)
            V(lambda e: e.tensor_tensor(out=sf[:], in0=pcs3, in1=trif_f.unsqueeze(1).to_broadcast([128, 4, 128]), op=ALU.mult),
              r=["pcs2", "cst"], w=[sfn])
            V(lambda e: e.tensor_tensor(out=sr[:], in0=pcs3, in1=trir_f.unsqueeze(1).to_broadcast([128, 4, 128]), op=ALU.mult),
              r=["pcs2", "cst"], w=[srn])
            yield
            for d in range(2):
                smt, smn = (sf, sfn) if d == 0 else (sr, srn)
                vt, vn = vts[d]
                snap4, snn = (snapF4, "snapF") if d == 0 else (snapR4, "snapR")
                for h in range(4):
                    T(lambda e: e.matmul(pn[:, d * 4 + h, 0:129], lhsT=smt[:, h, :], rhs=vt[:, h, :], start=True, stop=False),
                      r=[smn, vn], w=["pn2"])
                    T(lambda e: e.matmul(pn[:, d * 4 + h, 0:129], lhsT=qt[:, h, :], rhs=snap4[:, c, h, :], start=False, stop=True),
                      r=[qtn, snn], w=["pn2"])
            st_, stn = sm.next()
            V(lambda e: e.tensor_tensor(out=st_[:, 0:8], in0=pn[:, :, 128], in1=alpha, op=ALU.mult), r=["pn2", gn], w=[stn])
            V(lambda e: e.tensor_scalar(out=st_[:, 16:24], in0=st_[:, 0:8], scalar1=1.0, scalar2=None, op0=ALU.max), r=[stn], w=[stn])
            V(lambda e: e.scalar_tensor_tensor(out=st_[:, 8:16], in0=st_[:, 0:8], scalar=-1.0, in1=st_[:, 16:24], op0=ALU.mult, op1=ALU.max),
              r=[stn], w=[stn])
            V(lambda e: e.reciprocal(out=st_[:, 16:24], in_=st_[:, 8:16]), r=[stn], w=[stn])
            V(lambda e: e.tensor_tensor(out=st_[:, 24:32], in0=st_[:, 16:24], in1=alpha, op=ALU.mult), r=[stn, gn], w=[stn])
            h0, h0n = wk.next()
            h1, h1n = wk.next()
            h03 = h0[:].rearrange("p (h v) -> p h v", h=4)
            h13 = h1[:].rearrange("p (h v) -> p h v", h=4)
            V(lambda e: e.tensor_tensor(out=h03, in0=pn[:, 0:4, 0:128], in1=bc_mid(st_[:, 24:28], 128), op=ALU.mult),
              r=["pn2", stn], w=[h0n])
            V(lambda e: e.tensor_tensor(out=h13, in0=pn[:, 4:8, 0:128], in1=bc_mid(st_[:, 28:32], 128), op=ALU.mult),
              r=["pn2", stn], w=[h1n])
            V(lambda e: e.tensor_tensor(out=h0[:], in0=h0[:], in1=h1[:], op=ALU.add), r=[h0n, h1n], w=[h0n])
            A(lambda e: e.activation(out=h1[:], in_=h0[:], func=AF.Square), r=[h0n], w=[h1n])
            s2, s2n = sm.next()
            V(lambda e: e.tensor_reduce(out=s2[:, 0:4], in_=h13, axis=AX.X, op=ALU.add), r=[h1n], w=[s2n])
            A(lambda e: e.activation(out=s2[:, 4:8], in_=s2[:, 0:4], func=AF.Ln, scale=1.0 / 128, bias=epsb[:, 0:1]),
              r=[s2n, "epsb"], w=[s2n])
            A(lambda e: e.activation(out=s2[:, 8:12], in_=s2[:, 4:8], func=AF.Exp, scale=-0.5), r=[s2n], w=[s2n])
            V(lambda e: e.tensor_tensor(out=h03, in0=h03, in1=bc_mid(s2[:, 8:12], 128), op=ALU.mult), r=[h0n, s2n], w=[h0n])
            mt, mtn = mb.next()
            V(lambda e: e.tensor_tensor(out=mt[:], in0=h0[:], in1=ogt[:], op=ALU.mult), r=[h0n, ogn], w=[mtn])
            if dbg:
                DMA(lambda e: e.dma_start(out=d_m[c * 128:(c + 1) * 128, :], in_=h0[:]), r=[h0n])
            for kc in range(4):
                T(lambda e: e.transpose(out=pt[:, kc, :], in_=mt[:, kc * 128:(kc + 1) * 128], identity=ident_b),
                  r=[mtn, "cstb"], w=["pt2"])
            V(lambda e: e.tensor_copy(out=mT_all[:, :, c * 128:(c + 1) * 128], in_=pt[:, 0:4, :]), r=["pt2"], w=["mT_all"])
            aps, apn = proj(hT, hn, C_AQ, 512)
            qr, qrn = mb.next()
            qk_norm_rope(aps, apn, 2, 4, qg_bc, "qg_bc", e_idx, qr[:], qrn)
            for j in range(4):
                T(lambda e: e.transpose(out=pt[:, 4 + j, :], in_=qr[:, j * 128:(j + 1) * 128], identity=ident_b),
                  r=[qrn, "cstb"], w=["pt2"])
            qa, qan = qTa.next()
            V(lambda e: e.tensor_copy(out=qa[:], in_=pt[:, 4:8, :]), r=["pt2"], w=[qan])
            qas[c] = (qa, qan)

        def attention(c, qa, qan):
            kts = [c, c + 1, c + 2, 18, 19]
            ps_list = {}
            for g in range(2):
                for ki, kt in enumerate(kts):
                    i = pabi[0] % 2
                    pabi[0] += 1
                    nm = "pab2_%d" % i
                    outp = pab[:, i * 512:(i + 1) * 512]
                    T(lambda e: e.matmul(outp, lhsT=kT_all[g * 64:(g + 1) * 64, kt, :],
                                         rhs=qa[g * 64:(g + 1) * 64, :, :].rearrange("p j t -> p (j t)"), start=True, stop=True),
                      r=["kT_all", qan], w=[nm])
                    p_, pn_ = pr.next()
                    A(lambda e: e.activation(out=p_[:].rearrange("p j t -> p (j t)"), in_=outp, func=AF.Exp, scale=0.125),
                      r=[nm], w=[pn_])
                    if ki == 0 or ki == 2:
                        if ki == 0:
                            mk = amask_b[:, 0:128] if c == 0 else trir_b
                        else:
                            mk = amask_b[:, 128:256] if c == NOWN - 1 else trif_b
                        V(lambda e: e.tensor_tensor(out=p_[:], in0=p_[:], in1=mk.unsqueeze(1).to_broadcast([128, 4, 128]),
                                                    op=ALU.mult), r=[pn_, "cstb", "amask_b"], w=[pn_])
                    ps_list[(g, ki)] = (p_, pn_)
            yield
            for g in range(2):
                for j in range(4):
                    hq = g * 4 + j
                    for ki, kt in enumerate(kts):
                        p_, pn_ = ps_list[(g, ki)]
                        T(lambda e: e.matmul(pn[:, hq, 0:65], lhsT=p_[:, j, :], rhs=v_all[:, kt, g, :], start=(ki == 0), stop=(ki == 4)),
                          r=[pn_, "v_all"], w=["pn2"])
            st_, stn = sm.next()
            V(lambda e: e.tensor_tensor(out=st_[:, 0:8], in0=pn[:, :, 64], in1=esink[:], op=ALU.add), r=["pn2", "esink"], w=[stn])
            V(lambda e: e.reciprocal(out=st_[:, 8:16], in_=st_[:, 0:8]), r=[stn], w=[stn])
            at, atn = mb.next()
            V(lambda e: e.tensor_tensor(out=at[:].rearrange("p (h d) -> p h d", h=8), in0=pn[:, :, 0:64], in1=bc_mid(st_[:, 8:16], 64),
                                        op=ALU.mult), r=["pn2", stn], w=[atn])
            if dbg:
                DMA(lambda e: e.dma_start(out=d_a[c * 128:(c + 1) * 128, :], in_=at[:]), r=[atn], q="pool")
            for kc in range(4):
                T(lambda e: e.transpose(out=pt[:, kc, :], in_=at[:, kc * 128:(kc + 1) * 128], identity=ident_b),
                  r=[atn, "cstb"], w=["pt2"])
            V(lambda e: e.tensor_copy(out=aT_all[:, :, c * 128:(c + 1) * 128], in_=pt[:, 0:4, :]), r=["pt2"], w=["aT_all"])

        for i in range(2):
            hT, hn, xt, xn = make_hT(K, xpre[i * 128:(i + 1) * 128, :], 1, R, "pt2", pt)
            attn_kv(hT, hn, 18 + i, None)
        qas = {}
        for e_idx in range(NEXT):
            hT, hn, xt, xn = make_hT(K, xown[e_idx * 128:(e_idx + 1) * 128, :], 0, R, "pt2", pt)
            attn_kv(hT, hn, e_idx, e_idx)
            go = own_part(e_idx - 1, hT, hn, e_idx) if 1 <= e_idx <= NOWN else None
            ga = None
            if e_idx >= 2:
                qa, qan = qas.pop(e_idx - 2)
                ga = attention(e_idx - 2, qa, qan)
            if go is not None:
                next(go)
            if ga is not None:
                next(ga)
            if go is not None:
                for _ in go:
                    pass
            if ga is not None:
                for _ in ga:
                    pass
    S.flush()
    scA.close()
    K.scope = K.root
    S.barrier()

    S.set_reorder("2b" in REORDER_PHASES)
    with phase_scope(K) as sc3:
        K.scope = sc3
        wG = K.sb("wG", [128, 8, 2048], BF16)
        wbm = K.sb("wbm", [128, 4, D], BF16)
        wba = K.sb("wba", [128, 4, D], BF16)
        wo = K.sb("wo", [128, 8, D], BF16)
        stg = Ring(K, "stg3", [128, 2048], F32, 2)
        load_w_bf16(wG, "wG", w_in, 8, C_MG, DIN, stg, dst_c0=0)
        load_w_bf16(wbm, "wbm", w_bm, 4, 0, D, stg)
        load_w_bf16(wba, "wba", w_ba, 4, 0, D, stg)
        load_w_bf16(wo, "wo", w_out, 8, 0, D, stg)
        g1bc = K.sb("g1bc", [128, D], F32)
        DMA(lambda e: e.dma_start(out=g1bc[:], in_=modscr[0:1, 2 * D:3 * D].partition_broadcast(128)), r=["modscr"], w=["g1bc"])
        R = {"xt": Ring(K, "xt3", [128, D], F32, 3), "ss": Ring(K, "ss3", [128, 4], F32, 3),
             "xs": Ring(K, "xs3", [128, D], BF16, 2), "hT": Ring(K, "hT3", [128, 8, 128], BF16, 2),
             "junk": K.sb("junk3", [128, D], BF16)}
        sg = Ring(K, "sg", [128, 2048], BF16, 2)
        t1r = Ring(K, "t1r", [128, D], F32, 2)
        zb = Ring(K, "zb", [128, D], BF16, 2)
        zT = Ring(K, "zT", [128, 8, 128], BF16, 2)
        x1r = Ring(K, "x1r", [128, D], F32, 2)
        pt = K.ps("pt3", [128, 8, 128], BF16)
        pab = K.ps("pab3", [128, 1024], F32)
        pn = K.ps("pn3", [128, 2048], F32)
        st2b = {}

        def stA(c):
            hT, hn, xt, xn = make_hT(K, xown[(c + 1) * 128:(c + 2) * 128, :], 0, R, "pt3", pt)
            sgt, sgn = sg.next()
            for q in range(4):
                i = q % 2
                nm = "pab3_%d" % i
                outp = pab[:, i * 512:(i + 1) * 512]
                for kc in range(8):
                    T(lambda e: e.matmul(outp, lhsT=hT[:, kc, :], rhs=wG[:, kc, q * 512:(q + 1) * 512], start=(kc == 0), stop=(kc == 7)),
                      r=[hn, "wG"], w=[nm])
                A(lambda e: e.activation(out=sgt[:, q * 512:(q + 1) * 512], in_=outp, func=AF.Sigmoid), r=[nm], w=[sgn])
            st2b[c] = (sgt, sgn, xt, xn)

        def stB(c):
            sgt, sgn, xt, xn = st2b.pop(c)
            for half in range(2):
                for kc in range(4):
                    T(lambda e: e.matmul(pn[:, half * 512:(half + 1) * 512], lhsT=mT_all[:, kc, c * 128:(c + 1) * 128],
                                         rhs=wbm[:, kc, half * 512:(half + 1) * 512], start=(kc == 0), stop=(kc == 3)),
                      r=["mT_all", "wbm"], w=["pn3_m"])
            for half in range(2):
                for kc in range(4):
                    T(lambda e: e.matmul(pn[:, 1024 + half * 512:1024 + (half + 1) * 512], lhsT=aT_all[:, kc, c * 128:(c + 1) * 128],
                                         rhs=wba[:, kc, half * 512:(half + 1) * 512], start=(kc == 0), stop=(kc == 3)),
                      r=["aT_all", "wba"], w=["pn3_a"])
            t1, t1n = t1r.next()
            V(lambda e: e.tensor_tensor(out=t1[:], in0=pn[:, 0:1024], in1=sgt[:, 0:1024], op=ALU.mult), r=["pn3_m", sgn], w=[t1n])
            t2, t2n = t1r.next()
            V(lambda e: e.tensor_tensor(out=t2[:], in0=pn[:, 1024:2048], in1=sgt[:, 1024:2048], op=ALU.mult), r=["pn3_a", sgn], w=[t2n])
            z, zn = zb.next()
            V(lambda e: e.tensor_tensor(out=z[:], in0=t1[:], in1=t2[:], op=ALU.add), r=[t1n, t2n], w=[zn])
            for kc in range(8):
                T(lambda e: e.transpose(out=pt[:, kc, :], in_=z[:, kc * 128:(kc + 1) * 128], identity=ident_b), r=[zn, "cstb"], w=["pt3"])
            zt, ztn = zT.next()
            A(lambda e: e.copy(out=zt[:], in_=pt[:]), r=["pt3"], w=[ztn])
            for half in range(2):
                for kc in range(8):
                    T(lambda e: e.matmul(pn[:, half * 512:(half + 1) * 512], lhsT=zt[:, kc, :], rhs=wo[:, kc, half * 512:(half + 1) * 512],
                                         start=(kc == 0), stop=(kc == 7)), r=[ztn, "wo"], w=["pn3_m"])
            x1, x1n = x1r.next()
            V(lambda e: e.tensor_tensor(out=x1[:], in0=pn[:, 0:1024], in1=g1bc[:], op=ALU.mult), r=["pn3_m", "g1bc"], w=[x1n])
            V(lambda e: e.tensor_tensor(out=x1[:], in0=x1[:], in1=xt[:], op=ALU.add), r=[x1n, xn], w=[x1n])
            DMA(lambda e: e.dma_start(out=x1scr[c * 128:(c + 1) * 128, :], in_=x1[:]), r=[x1n], w=["x1scr"])

        stA(0)
        for c in range(NOWN):
            if c + 1 < NOWN:
                stA(c + 1)
            stB(c)
    S.flush()
    scB.close()
    K.scope = K.root
    S.barrier()

    if dbg == 2:
        S.finish()
        K.root.close()
        return nc

    TB = 256
    NTB = NOWN * 128 // TB
    scP = contextlib.ExitStack()
    K.scope = scP
    i1T_all = K.sb("i1T_all", [128, NOWN * 128], BF16)
    i2T_all = K.sb("i2T_all", [128, NOWN * 128], BF16)
    gT_all = K.sb("gT_all", [128, NOWN * 128], BF16)
    iota128 = K.sb("iota128", [128, 128], F32)
    G(lambda e: e.iota(iota128[:], pattern=[[1, 128]], base=0, channel_multiplier=0, allow_small_or_imprecise_dtypes=True), w=["iota128"])

    S.set_reorder("3.1" in REORDER_PHASES)
    with phase_scope(K) as sc4:
        K.scope = sc4
        wq = K.sb("wq", [128, 8, 2048], BF16)
        stg = Ring(K, "stg4", [128, 1024], F32, 2)
        load_w_bf16(wq, "wq", w_q, 8, 0, 2048, stg)
        skT = K.sb("skT", [128, 16, 128], BF16)
        g2p = K.sb("g2p", [128, D], F32)
        sh2bc = K.sb("sh2bc", [128, D], F32)
        DMA(lambda e: e.dma_start(out=sh2bc[:], in_=modscr[0:1, 3 * D:4 * D].partition_broadcast(128)), r=["modscr"], w=["sh2bc"])
        DMA(lambda e: e.dma_start(out=g2p[:], in_=modscr[0:1, 4 * D:5 * D].partition_broadcast(128)), r=["modscr"], w=["g2p"])
        DMA(lambda e: e.dma_start(out=stg.t[0][:, 0:D], in_=nfg_d[0:1, :].partition_broadcast(128)), w=[stg.names[0]])
        V(lambda e: e.scalar_tensor_tensor(out=g2p[:], in0=g2p[:], scalar=1.0, in1=stg.t[0][:, 0:D], op0=ALU.add, op1=ALU.mult),
          r=["g2p", stg.names[0]], w=["g2p"])
        pt = K.ps("pt4", [128, 8, 128], BF16)
        pq = K.ps("pq4", [128, 2048], F32)
        ptf = K.ps("ptf4", [128, 512], F32)
        x1t = Ring(K, "x1t", [128, D], F32, 2)
        ssr = Ring(K, "ss4", [128, 4], F32, 2)
        junk = K.sb("junk4", [128, D], BF16)
        h2 = Ring(K, "h2", [128, D], F32, 2)
        h2b = Ring(K, "h2b", [128, D], BF16, 2)
        h2Tr = Ring(K, "h2Tr", [128, 8, 128], BF16, 2)
        qT = Ring(K, "qT", [128, 16, 128], BF16, 2)
        s_sbr = Ring(K, "s_sb", [128, 16, 128], F32, 2)
        sk_f, sk_b, sk_bn = s_sbr.t[0], qT.t[0], qT.names[0]
        DMA(lambda e: e.dma_start(out=sk_f[:], in_=skeys.rearrange("(hp n) d -> n hp d", n=128)), w=[s_sbr.names[0]])
        V(lambda e: e.tensor_copy(out=sk_b[:], in_=sk_f[:]), r=[s_sbr.names[0]], w=[sk_bn])
        for hp in range(16):
            T(lambda e: e.transpose(out=pt[:, hp % 8, :], in_=sk_b[:, hp, :], identity=ident_b), r=[sk_bn, "cstb"], w=["pt4"])
            V(lambda e: e.tensor_copy(out=skT[:, hp, :], in_=pt[:, hp % 8, :]), r=["pt4"], w=["skT"])
        s_wk = K.sb("s_wk", [128, 16, 128], F32)
        cwk = s_wk[:].rearrange("p a b -> p (a b)").rearrange("p (h c) -> p h c", h=8)
        tv = K.sb("tv", [128, 16, 16], F32)
        tix = K.sb("tix", [128, 16, 16], U32)
        tixf = K.sb("tixf", [128, 16, 16], F32)
        cand = K.sb("cand", [128, 8, 256], F32)
        cv = K.sb("cv", [128, 8, 16], F32)
        cpos = K.sb("cpos", [128, 8, 16], U32)
        cj = K.sb("cj", [128, 2, 128], U32)
        cjf = K.sb("cjf", [128, 2, 128], F32)
        oh = K.sb("oh", [128, 128, 16], F32)
        oh2 = K.sb("oh2", [128, 128, 16], F32)
        iif = K.sb("iif", [128, 2, 128], F32)
        gw = K.sb("gw", [128, 8, 16], F32)
        gst = K.sb("gst", [128, 32], F32)
        NEG = -3.0e38
        def route_tile(it):
            xt, xn = x1t.next()
            DMA(lambda e: e.dma_start(out=xt[:], in_=x1scr[it * 128:(it + 1) * 128, :]), r=["x1scr"], w=[xn])
            ss, ssn = ssr.next()
            A(lambda e: e.activation(out=junk[:], in_=xt[:], func=AF.Square, accum_out=ss[:, 0:1]), r=[xn], w=["junk4", ssn])
            A(lambda e: e.activation(out=ss[:, 1:2], in_=ss[:, 0:1], func=AF.Ln, scale=1.0 / D, bias=epsb[:, 0:1]), r=[ssn, "epsb"], w=[ssn])
            A(lambda e: e.activation(out=ss[:, 2:3], in_=ss[:, 1:2], func=AF.Exp, scale=-0.5), r=[ssn], w=[ssn])
            ht, htn = h2.next()
            V(lambda e: e.scalar_tensor_tensor(out=ht[:], in0=xt[:], scalar=ss[:, 2:3], in1=g2p[:], op0=ALU.mult, op1=ALU.mult),
              r=[xn, ssn, "g2p"], w=[htn])
            hb, hbn = h2b.next()
            V(lambda e: e.tensor_tensor(out=hb[:], in0=ht[:], in1=sh2bc[:], op=ALU.add), r=[htn, "sh2bc"], w=[hbn])
            for kc in range(8):
                T(lambda e: e.transpose(out=pt[:, kc, :], in_=hb[:, kc * 128:(kc + 1) * 128], identity=ident_b), r=[hbn, "cstb"], w=["pt4"])
            hTt, hTtn = h2Tr.next()
            A(lambda e: e.copy(out=hTt[:], in_=pt[:]), r=["pt4"], w=[hTtn])
            DMA(lambda e: e.dma_start(out=h2T_scr[it].rearrange("p (k t) -> p k t", k=8), in_=hTt[:]), r=[hTtn], w=["h2T_scr"])
            for hp in range(16):
                for kc in range(8):
                    T(lambda e: e.matmul(pq[:, hp * 128:(hp + 1) * 128], lhsT=wq[:, kc, hp * 128:(hp + 1) * 128], rhs=hTt[:, kc, :],
                                         start=(kc == 0), stop=(kc == 7)), r=[hTtn, "wq"], w=["pq4"])
            qt, qtn = qT.next()
            A(lambda e: e.copy(out=qt[:], in_=pq[:].rearrange("p (a b) -> p a b", a=16)), r=["pq4"], w=[qtn])
            for hp in range(16):
                T(lambda e: e.matmul(pq[:, hp * 128:(hp + 1) * 128], lhsT=qt[:, hp, :], rhs=skT[:, hp, :], start=True, stop=True),
                  r=[qtn, "skT"], w=["pq4"])
            s_sb, s_sbn = s_sbr.next()
            A(lambda e: e.copy(out=s_sb[:], in_=pq[:].rearrange("p (a b) -> p a b", a=16)), r=["pq4"], w=[s_sbn])
            yield
            for hp in range(16):
                V(lambda e: e.max(out=tv[:, hp, 0:8], in_=s_sb[:, hp, :]), r=[s_sbn], w=["tv"])
                V(lambda e: e.max_index(out=tix[:, hp, 0:8], in_max=tv[:, hp, 0:8], in_values=s_sb[:, hp, :]), r=[s_sbn, "tv"], w=["tix"])
                V(lambda e: e.match_replace(out=s_wk[:, hp, :], in_to_replace=tv[:, hp, 0:8], in_values=s_sb[:, hp, :], imm_value=NEG),
                  r=[s_sbn, "tv"], w=["s_wk"])
                V(lambda e: e.max(out=tv[:, hp, 8:16], in_=s_wk[:, hp, :]), r=["s_wk"], w=["tv"])
                V(lambda e: e.max_index(out=tix[:, hp, 8:16], in_max=tv[:, hp, 8:16], in_values=s_wk[:, hp, :]), r=["s_wk", "tv"], w=["tix"])
            V(lambda e: e.tensor_copy(out=tixf[:], in_=tix[:]), r=["tix"], w=["tixf"])
            tv4 = tv[:].rearrange("p (h two) k -> p h two k", two=2)
            cand4 = cand[:].rearrange("p h (a b) -> p h a b", a=16)
            V(lambda e: e.tensor_tensor(out=cand4, in0=tv4[:, :, 0, :].unsqueeze(3).to_broadcast([128, 8, 16, 16]),
                                        in1=tv4[:, :, 1, :].unsqueeze(2).to_broadcast([128, 8, 16, 16]), op=ALU.add),
              r=["tv"], w=["cand"])
            for hh in range(8):
                V(lambda e: e.max(out=cv[:, hh, 0:8], in_=cand[:, hh, :]), r=["cand"], w=["cv"])
                V(lambda e: e.max_index(out=cpos[:, hh, 0:8], in_max=cv[:, hh, 0:8], in_values=cand[:, hh, :]), r=["cand", "cv"], w=["cpos"])
                V(lambda e: e.match_replace(out=cwk[:, hh, :], in_to_replace=cv[:, hh, 0:8], in_values=cand[:, hh, :], imm_value=NEG),
                  r=["cand", "cv"], w=["s_wk"])
                V(lambda e: e.max(out=cv[:, hh, 8:16], in_=cwk[:, hh, :]), r=["s_wk"], w=["cv"])
                V(lambda e: e.max_index(out=cpos[:, hh, 8:16], in_max=cv[:, hh, 8:16], in_values=cwk[:, hh, :]), r=["s_wk", "cv"], w=["cpos"])
            V(lambda e: e.tensor_tensor(out=gw[:], in0=cv[:], in1=bc_mid(cv[:, :, 0], 16), op=ALU.subtract), r=["cv"], w=["gw"])
            A(lambda e: e.activation(out=gw[:], in_=gw[:], func=AF.Exp), r=["gw"], w=["gw"])
            V(lambda e: e.tensor_reduce(out=gst[:, 0:8], in_=gw[:], axis=AX.X, op=ALU.add), r=["gw"], w=["gst"])
            V(lambda e: e.reciprocal(out=gst[:, 8:16], in_=gst[:, 0:8]), r=["gst"], w=["gst"])
            V(lambda e: e.tensor_tensor(out=gw[:], in0=gw[:], in1=bc_mid(gst[:, 8:16], 16), op=ALU.mult), r=["gw", "gst"], w=["gw"])
            cpf = cpos[:].rearrange("p h k -> p (h k)")
            V(lambda e: e.tensor_single_scalar(out=cj[:, 0, :], in_=cpf, scalar=4, op=ALU.logical_shift_right), r=["cpos"], w=["cj"])
            V(lambda e: e.tensor_single_scalar(out=cj[:, 1, :], in_=cpf, scalar=15, op=ALU.bitwise_and), r=["cpos"], w=["cj"])
            V(lambda e: e.tensor_copy(out=cjf[:], in_=cj[:]), r=["cj"], w=["cjf"])
            tixf4 = tixf[:].rearrange("p (h two) k -> p h two k", two=2)
            for two in range(2):
                V(lambda e: e.tensor_tensor(out=oh[:], in0=bc_mid(cjf[:, two, :], 16),
                                            in1=iota16.unsqueeze(1).to_broadcast([128, 128, 16]), op=ALU.is_equal),
                  r=["cjf", "cst"], w=["oh"])
                oh4 = oh[:].rearrange("p (h k) j -> p h k j", h=8)
                oh24 = oh2[:].rearrange("p (h k) j -> p h k j", h=8)
                V(lambda e: e.tensor_tensor(out=oh24, in0=oh4,
                                            in1=tixf4[:, :, two, :].unsqueeze(2).to_broadcast([128, 8, 16, 16]), op=ALU.mult),
                  r=["oh", "tixf"], w=["oh2"])
                V(lambda e: e.tensor_reduce(out=iif[:, two, :], in_=oh2[:], axis=AX.X, op=ALU.add), r=["oh2"], w=["iif"])
            T(lambda e: e.transpose(out=ptf[:, 0:128], in_=iif[:, 0, :], identity=ident_f), r=["iif", "cst"], w=["ptf4"])
            T(lambda e: e.transpose(out=ptf[:, 128:256], in_=iif[:, 1, :], identity=ident_f), r=["iif", "cst"], w=["ptf4"])
            T(lambda e: e.transpose(out=ptf[:, 256:384], in_=gw[:].rearrange("p h k -> p (h k)"), identity=ident_f), r=["gw", "cst"], w=["ptf4"])
            sl = slice(it * 128, (it + 1) * 128)
            A(lambda e: e.copy(out=i1T_all[:, sl], in_=ptf[:, 0:128]), r=["ptf4"], w=["i1T_all"])
            A(lambda e: e.copy(out=i2T_all[:, sl], in_=ptf[:, 128:256]), r=["ptf4"], w=["i2T_all"])
            A(lambda e: e.copy(out=gT_all[:, sl], in_=ptf[:, 256:384]), r=["ptf4"], w=["gT_all"])

        rgen = {0: route_tile(0)}
        next(rgen[0])
        for it in range(NOWN):
            if it + 1 < NOWN:
                rgen[it + 1] = route_tile(it + 1)
                next(rgen[it + 1])
            for _ in rgen.pop(it):
                pass
    K.scope = scP
    S.barrier()

    S.set_reorder("3.2" in REORDER_PHASES)
    with phase_scope(K) as sc5:
        K.scope = sc5
        g2bc = K.sb("g2bc", [128, D], F32)
        DMA(lambda e: e.dma_start(out=g2bc[:], in_=modscr[0:1, 5 * D:6 * D].partition_broadcast(128)), r=["modscr"], w=["g2bc"])
        WTs = [K.sb("WT%d" % i, [128, TB, 128], BF16) for i in range(2)]
        SUBT = 8
        oh1 = Ring(K, "oh1_", [128, SUBT, 128], BF16, 3)
        oh2r = Ring(K, "oh2_", [128, SUBT, 128], BF16, 3)
        utr = Ring(K, "utr", [128, 2, 8, 128], BF16, 3)
        vr = Ring(K, "vr", [128, 2, D], BF16, 3)
        gt_r = Ring(K, "gt_r", [128, 2, TB], BF16, 2)
        pt_r = Ring(K, "pt_r", [128, 2, TB], BF16, 2)
        h2Tb = Ring(K, "h2Tb", [128, 8, TB], BF16, 2)
        x1t = Ring(K, "x1u", [128, 256], F32, 1)
        outt = Ring(K, "outu", [128, 256], F32, 1)
        pacc = K.ps("pacc", [128, 2, D], F32)
        pst = [K.ps("pst%d" % i, [128, 2, TB], F32) for i in range(2)]
        pw = [K.ps("pw%d" % i, [128, 4, 128], F32) for i in range(2)]
        iota_bc = iota128[:].unsqueeze(1).to_broadcast([128, SUBT, 128])
        NSUB = TB // SUBT
        pwc = [0]

        wb_state = {}

        def wbuild_gen(tb, sub):
            ts0 = tb * TB + sub * SUBT
            o1, o1n = oh1.next()
            o2, o2n = oh2r.next()
            V(lambda e: e.tensor_tensor(out=o1[:], in0=iota_bc, in1=bc_mid(i1T_all[:, ts0:ts0 + SUBT], 128), op=ALU.is_equal),
              r=["iota128", "i1T_all"], w=[o1n])
            V(lambda e: e.tensor_tensor(out=o2[:], in0=iota_bc, in1=bc_mid(i2T_all[:, ts0:ts0 + SUBT], 128), op=ALU.is_equal),
              r=["iota128", "i2T_all"], w=[o2n])
            G(lambda e: e.tensor_tensor(out=o2[:], in0=o2[:], in1=bc_mid(gT_all[:, ts0:ts0 + SUBT], 128), op=ALU.mult),
              r=[o2n, "gT_all"], w=[o2n])
            wb_state[(tb, sub)] = (o1, o1n, o2, o2n)

        def wbuild_mm(tb, sub):
            WT = WTs[tb % 2]
            wn = "WT%d" % (tb % 2)
            o1, o1n, o2, o2n = wb_state.pop((tb, sub))
            for q4 in range(SUBT // 4):
                pwi = pw[pwc[0] % 2]
                pwn = "pw%d" % (pwc[0] % 2)
                pwc[0] += 1
                for tt in range(4):
                    tl = q4 * 4 + tt
                    T(lambda e: e.matmul(pwi[:, tt, :], lhsT=o2[:, tl, :], rhs=o1[:, tl, :], start=True, stop=True),
                      r=[o1n, o2n], w=[pwn])
                tw0 = sub * SUBT + q4 * 4
                A(lambda e: e.copy(out=WT[:, tw0:tw0 + 4, :], in_=pwi[:]), r=[pwn], w=[wn])

        def wbuild_sub(tb, sub):
            wbuild_gen(tb, sub)
            wbuild_mm(tb, sub)

        def load_h2T(tb):
            hb_, hbn_ = h2Tb.next()
            for half in range(2):
                DMA(lambda e: e.dma_start(out=hb_[:, :, half * 128:(half + 1) * 128],
                                          in_=h2T_scr[tb * 2 + half].rearrange("p (k t) -> p k t", k=8)), r=["h2T_scr"], w=[hbn_])
            return hb_, hbn_

        for sub in range(NSUB):
            wbuild_sub(0, sub)
        hcur = load_h2T(0)
        for tb in range(NTB):
            t0 = tb * TB
            WT = WTs[tb % 2]
            wtn = "WT%d" % (tb % 2)
            h2T_blk, h2T_bn = hcur
            if tb + 1 < NTB:
                hcur = load_h2T(tb + 1)

            def sweep_load(pr_):
                ut_, utn = utr.next()
                DMA(lambda e: e.dma_start(out=ut_[:], in_=UT_scr[pr_].rearrange("p (c k e) -> p c k e", c=2, k=8)), r=["UT_scr"], w=[utn])
                vv, vvn = vr.next()
                DMA(lambda e: e.dma_start(out=vv[:], in_=V_scr[pr_].rearrange("p (c d) -> p c d", c=2)), r=["V_scr"], w=[vvn], q="act")
                return ut_, utn, vv, vvn

            def sweep_mm1(pr_, ut_, utn):
                ps_ = pst[pr_ % 2]
                psn = "pst%d" % (pr_ % 2)
                for ci in range(2):
                    for kc in range(8):
                        T(lambda e: e.matmul(ps_[:, ci, :], lhsT=ut_[:, ci, kc, :], rhs=h2T_blk[:, kc, :],
                                             start=(kc == 0), stop=(kc == 7)), r=[utn, h2T_bn], w=[psn])
                gt, gtn = gt_r.next()
                A(lambda e: e.activation(out=gt[:], in_=ps_[:], func=AF.Gelu), r=[psn], w=[gtn])
                pt_, ptn = pt_r.next()
                V(lambda e: e.tensor_tensor(out=pt_[:], in0=gt[:], in1=WT[:, :, pr_ * 2:pr_ * 2 + 2].rearrange("p t i -> p i t"), op=ALU.mult),
                  r=[gtn, wtn], w=[ptn])
                return pt_, ptn

            def sweep_mm2(pr_, pt_, ptn, vv, vvn):
                for ci in range(2):
                    for ts in range(2):
                        for dh in range(2):
                            T(lambda e: e.matmul(pacc[:, ts, dh * 512:(dh + 1) * 512], lhsT=pt_[:, ci, ts * 128:(ts + 1) * 128],
                                                 rhs=vv[:, ci, dh * 512:(dh + 1) * 512], start=(pr_ == 0 and ci == 0),
                                                 stop=(pr_ == 63 and ci == 1)), r=[ptn, vvn], w=["pacc"])

            lds = {0: sweep_load(0), 1: sweep_load(1)}
            pts = {0: sweep_mm1(0, lds[0][0], lds[0][1])}
            for pr_ in range(64):
                if pr_ + 2 < 64:
                    lds[pr_ + 2] = sweep_load(pr_ + 2)
                if pr_ + 1 < 64:
                    pts[pr_ + 1] = sweep_mm1(pr_ + 1, lds[pr_ + 1][0], lds[pr_ + 1][1])
                pt_, ptn = pts.pop(pr_)
                ut_, utn, vv, vvn = lds.pop(pr_)
                sweep_mm2(pr_, pt_, ptn, vv, vvn)
                if tb + 1 < NTB and pr_ % 2 == 1:
                    wbuild_gen(tb + 1, pr_ // 2)
                    if pr_ // 2 >= 1:
                        wbuild_mm(tb + 1, pr_ // 2 - 1)
            if tb + 1 < NTB:
                wbuild_mm(tb + 1, NSUB - 1)
            for ts in range(2):
                row0 = t0 + ts * 128
                for dh in range(4):
                    cs_ = slice(dh * 256, (dh + 1) * 256)
                    xt, xn = x1t.next()
                    DMA(lambda e: e.dma_start(out=xt[:], in_=x1scr[row0:row0 + 128, cs_]), r=["x1scr"], w=[xn])
                    ot, otn = outt.next()
                    V(lambda e: e.tensor_tensor(out=ot[:], in0=pacc[:, ts, cs_], in1=g2bc[:, cs_], op=ALU.mult), r=["pacc", "g2bc"], w=[otn])
                    V(lambda e: e.tensor_tensor(out=ot[:], in0=ot[:], in1=xt[:], op=ALU.add), r=[otn, xn], w=[otn])
                    DMA(lambda e: e.dma_start(out=out_d[row0:row0 + 128, cs_], in_=ot[:]), r=[otn], w=["out_d"])
    S.flush()
    scP.close()
    K.scope = K.root
    S.finish()
    K.root.close()
    return nc


def _host_inputs(inputs):
    f = lambda a: np.ascontiguousarray(np.asarray(a, dtype=np.float32))
    x, c, ctx, c_ctx = f(inputs["x"]), f(inputs["c"]), f(inputs["ctx"]), f(inputs["c_ctx"])
    T_ = x.shape[1]
    consts = np.zeros((128, 400), np.float32)
    r = np.arange(128)
    consts[:, 0:128] = np.eye(128, dtype=np.float32)
    consts[:, 128:256] = (r[:, None] <= r[None, :])
    consts[:, 256:384] = (r[:, None] >= r[None, :])
    consts[:, 384:400] = np.arange(16, dtype=np.float32)[None, :]
    inv = (10000.0 ** (-np.arange(16, dtype=np.float32) / 16)).astype(np.float32)
    shared = {
        "consts": consts,
        "nmgT": f(inputs["norm_mix_g"]).reshape(8, 128).T.copy(),
        "w_ada": f(inputs["w_ada"])[0], "b_ada": f(inputs["b_ada"]).reshape(1, -1),
        "norm_ffn_g": f(inputs["norm_ffn_g"]).reshape(1, -1), "w_in": f(inputs["w_in"])[0],
        "b_mgates": f(inputs["b_mgates"]).reshape(1, -1), "mlstm_norm_g": f(inputs["mlstm_norm_g"]).reshape(1, -1),
        "attn_q_norm_g": f(inputs["attn_q_norm_g"]).reshape(1, -1), "attn_k_norm_g": f(inputs["attn_k_norm_g"]).reshape(1, -1),
        "attn_sink": f(inputs["attn_sink"]).reshape(1, -1), "w_branch_m": f(inputs["w_branch_m"])[0],
        "w_branch_a": f(inputs["w_branch_a"])[0], "w_out": f(inputs["w_out"])[0],
        "peer_w_query": f(inputs["peer_w_query"])[0], "peer_sub_keys": f(inputs["peer_sub_keys"]).reshape(16 * 128, 128),
        "peer_u": f(inputs["peer_u"])[0], "peer_v": f(inputs["peer_v"])[0],
    }
    in_maps = []
    for core in range(8):
        b, j = core // 4, core % 4
        s0 = 2048 * j
        xo = np.zeros((NEXT * 128, D), np.float32)
        lo, hi = max(0, s0 - 128), min(T_, s0 + 2048 + 128)
        xo[lo - (s0 - 128):hi - (s0 - 128)] = x[b, lo:hi]
        xp = np.zeros((NPRE * 128, D), np.float32)
        pm = np.zeros((NPRE, 2), np.float32)
        xp[0:128], xp[128:256] = ctx[b, 0:128], ctx[b, 128:256]
        xp[256:384], xp[384:512] = ctx[b, 128:256], ctx[b, 0:128]
        pm[0], pm[1], pm[2], pm[3] = (1, 0), (1, 0), (0, 1), (0, 1)
        slot = 4
        for ch in range(0, 16 * j):
            xp[slot * 128:(slot + 1) * 128] = x[b, ch * 128:(ch + 1) * 128]
            pm[slot] = (1, 0)
            slot += 1
        for ch in range(63, 16 * (j + 1) - 1, -1):
            xp[slot * 128:(slot + 1) * 128] = x[b, ch * 128:(ch + 1) * 128]
            pm[slot] = (0, 1)
            slot += 1
        assert slot == NPRE
        cT = np.stack([c[b].reshape(8, 128).T, c_ctx.reshape(8, 128).T], axis=-1).reshape(128, 16)
        pos = (s0 - 128) + np.arange(NEXT * 128)
        row, col = pos // 64, pos % 64
        ang = np.concatenate([row[:, None].astype(np.float32) * inv, col[:, None].astype(np.float32) * inv], axis=-1)
        cs = np.stack([np.cos(ang), np.sin(ang)], axis=1).astype(np.float32)
        rope = cs.reshape(NEXT, 128, 64).transpose(1, 0, 2).reshape(128, NEXT * 64)
        am = np.zeros((128, 256), np.float32)
        am[:, 0:128] = consts[:, 256:384] * (1.0 if j > 0 else 0.0)
        am[:, 128:256] = consts[:, 128:256] * (1.0 if j < 3 else 0.0)
        m = dict(shared)
        m.update({"xown": xo, "xpre": xp, "pmask": np.broadcast_to(pm.reshape(1, -1), (128, NPRE * 2)).copy(),
                  "cT": np.ascontiguousarray(cT), "rope": np.ascontiguousarray(rope), "amask": am})
        in_maps.append(m)
    return in_maps


_NC_CACHE = {}


def kernel(**inputs):
    in_maps = _host_inputs(inputs)
    if 0 not in _NC_CACHE:
        _NC_CACHE[0] = build(0)
    res = run_bass_kernel_spmd(_NC_CACHE[0], in_maps, core_ids=list(range(8)))
    outs = [np.asarray(r["out"], dtype=np.float32) for r in res.results]
    out = np.zeros((2, 8192, D), np.float32)
    for core in range(8):
        b, j = core // 4, core % 4
        out[b, 2048 * j:2048 * (j + 1)] = outs[core]
    return out
```
